# Optimizing a Trainium2 kernel written in Bass

```python
import math
import jax, jax.numpy as jnp
from jax import lax
import numpy as np

D_MODEL = 1024
BATCH = 4
SEQ = 8192
DEPTH = 1
DEC_BATCH = 16
DEC_SEQ = 64
PAST_LEN = 1024

CHUNK = 64
FOX_HEADS = 8
FOX_HEAD_DIM = 64
FOX_WIDTH = FOX_HEADS * FOX_HEAD_DIM
HGRN_HEADS = 4
HGRN_EXPAND = 128
HGRN_WIDTH = HGRN_HEADS * HGRN_EXPAND
HGRN_HEAD_V = HGRN_WIDTH // HGRN_HEADS
N_EXPERTS = 256
TOP_K = 8
N_GROUPS = 8
TOPK_GROUPS = 4
D_EXPERT = 256
D_SHARED = 256
ROUTED_SCALE = 2.5
Q_BLOCK = 128
NORM_EPS = 1e-6
IN_COLS = 3 * FOX_WIDTH + FOX_HEADS + 4 * HGRN_WIDTH + 2 * D_MODEL

kernel_name = 'fox_hgrn2_moe_adaln_stream_step'


def rmsnorm(x, g):
    xf = x.astype(jnp.float32)
    y = xf * lax.rsqrt(jnp.mean(xf * xf, axis=-1, keepdims=True) + NORM_EPS)
    return (y * g.astype(jnp.float32)).astype(x.dtype)


def split_cols(proj):
    sizes = [FOX_WIDTH, FOX_WIDTH, FOX_WIDTH, FOX_HEADS,
             HGRN_WIDTH, HGRN_WIDTH, HGRN_WIDTH, HGRN_WIDTH, D_MODEL, D_MODEL]
    idx = []
    acc = 0
    for s in sizes[:-1]:
        acc += s
        idx.append(acc)
    return jnp.split(proj, idx, axis=-1)


def fox_attention(q, k, v, fq, fk, q_offset):
    sq = q.shape[1]
    lk = k.shape[1]
    scale = FOX_HEAD_DIM ** -0.5
    outs = []
    for b0 in range(0, sq, Q_BLOCK):
        qn = min(Q_BLOCK, sq - b0)
        nk = min(lk, q_offset + b0 + qn)
        s = jnp.einsum('bqhd,bkhd->bhqk', q[:, b0:b0 + qn], k[:, :nk]).astype(jnp.float32) * scale
        bias = (jnp.transpose(fq[:, b0:b0 + qn], (0, 2, 1))[:, :, :, None]
                - jnp.transpose(fk[:, :nk], (0, 2, 1))[:, :, None, :])
        q_pos = q_offset + b0 + jnp.arange(qn)
        k_pos = jnp.arange(nk)
        mask = k_pos[None, :] <= q_pos[:, None]
        p = jax.nn.softmax(jnp.where(mask, s + bias, -jnp.inf), axis=-1).astype(v.dtype)
        outs.append(jnp.einsum('bhqk,bkhd->bqhd', p, v[:, :nk]))
    return jnp.concatenate(outs, axis=1)


def hgrn2_recurrence(q, k, i, logf, s0):
    bsz, seq, nh, dk = q.shape
    dv = i.shape[-1]
    L = min(CHUNK, seq)
    n = seq // L

    def to_blocks(a):
        return jnp.transpose(a.reshape(bsz, n, L, nh, a.shape[-1]), (1, 0, 3, 2, 4)).astype(jnp.float32)

    tri = jnp.tril(jnp.ones((L, L), dtype=bool))[:, :, None]

    def step(state, xs):
        qc, kc, ic, gc = xs
        b = jnp.cumsum(gc, axis=2)
        inter = jnp.einsum('bhtd,bhde->bhte', qc * jnp.exp(b), state)
        diff = b[:, :, :, None, :] - b[:, :, None, :, :]
        decay = jnp.exp(jnp.where(tri, diff, -jnp.inf))
        a = jnp.einsum('bhtd,bhsd,bhtsd->bhts', qc, kc, decay)
        o = inter + jnp.einsum('bhts,bhse->bhte', a, ic)
        b_last = b[:, :, -1:, :]
        new_state = (jnp.exp(b_last[:, :, 0, :])[..., None] * state
                     + jnp.einsum('bhsd,bhse->bhde', kc * jnp.exp(b_last - b), ic))
        return new_state, o

    s_fin, o = lax.scan(step, s0.astype(jnp.float32),
                        (to_blocks(q), to_blocks(k), to_blocks(i), to_blocks(logf)))
    o = jnp.transpose(o, (1, 0, 3, 2, 4)).reshape(bsz, seq, nh, dv)
    return o, s_fin


def swiglu(x, wg, wu, wd):
    return (jax.nn.silu(x @ wg) * (x @ wu)) @ wd


def routed_experts(x, idx, wts, w_gate, w_up, w_down):
    t, d = x.shape
    a = t * TOP_K
    blk = int(min(128, max(8, 2 ** int(math.log2(max(1, a // N_EXPERTS))))))
    n_blocks = -(-(a + N_EXPERTS * (blk - 1)) // blk)
    rows = n_blocks * blk
    e_flat = idx.reshape(-1)
    tok_flat = jnp.arange(a, dtype=jnp.int32) // TOP_K
    w_flat = wts.reshape(-1)
    order = jnp.argsort(e_flat)
    e_sorted = e_flat[order]
    counts = jnp.bincount(e_flat, length=N_EXPERTS)
    starts = jnp.cumsum(counts) - counts
    padded = (counts + blk - 1) // blk * blk
    pend = jnp.cumsum(padded)
    pstart = pend - padded
    dest = pstart[e_sorted] + jnp.arange(a) - starts[e_sorted]
    row_tok = jnp.full((rows,), t, dtype=jnp.int32).at[dest].set(tok_flat[order])
    row_w = jnp.zeros((rows,), jnp.float32).at[dest].set(w_flat[order])
    blk_expert = jnp.minimum(jnp.searchsorted(pend, jnp.arange(n_blocks) * blk, side='right'), N_EXPERTS - 1)
    x_pad = jnp.concatenate([x, jnp.zeros((1, d), x.dtype)], axis=0)

    def body(y, xs):
        tok, w, e = xs
        xb = x_pad[tok]
        yb = swiglu(xb, w_gate[e], w_up[e], w_down[e]) * w[:, None].astype(x.dtype)
        return y.at[tok].add(yb), None

    y, _ = lax.scan(body, jnp.zeros((t + 1, d), x.dtype),
                    (row_tok.reshape(n_blocks, blk), row_w.reshape(n_blocks, blk), blk_expert))
    return y[:t]


def moe_ffn(h, w_router, b_router, w_exp_gate, w_exp_up, w_exp_down, w_sh_gate, w_sh_up, w_sh_down):
    bsz, seq, d = h.shape
    x = h.reshape(bsz * seq, d)
    t = x.shape[0]
    scores = jax.nn.sigmoid((x @ w_router).astype(jnp.float32))
    biased = scores + b_router.astype(jnp.float32)
    gscore = lax.top_k(biased.reshape(t, N_GROUPS, N_EXPERTS // N_GROUPS), 2)[0].sum(-1)
    _, gidx = lax.top_k(gscore, TOPK_GROUPS)
    gmask = jax.nn.one_hot(gidx, N_GROUPS, dtype=jnp.float32).sum(1)
    emask = jnp.repeat(gmask, N_EXPERTS // N_GROUPS, axis=1) > 0
    _, idx = lax.top_k(jnp.where(emask, biased, -jnp.inf), TOP_K)
    wts = jnp.take_along_axis(scores, idx, axis=1)
    wts = wts / jnp.sum(wts, axis=-1, keepdims=True) * ROUTED_SCALE
    routed = routed_experts(x, idx, wts, w_exp_gate, w_exp_up, w_exp_down)
    shared = swiglu(x, w_sh_gate, w_sh_up, w_sh_down)
    return (routed + shared).reshape(bsz, seq, d)


def trunk_layer(x, c, past, lb, w_ada, b_ada, g_norm1, w_in, b_fox_f, g_q, g_k, g_hgrn_o,
                w_proj_a, w_proj_b, w_out, g_norm2, w_router, b_router,
                w_exp_gate, w_exp_up, w_exp_down, w_sh_gate, w_sh_up, w_sh_down):
    f32 = jnp.float32
    bsz, seq, _ = x.shape
    mod = (jax.nn.silu(c) @ w_ada + b_ada)[:, None, :]
    shift1, scale1, gate1, shift2, scale2, gate2 = jnp.split(mod, 6, axis=-1)
    h = rmsnorm(x, g_norm1) * (1 + scale1) + shift1
    fq, fk, fv, ff, hq, hf, hi, hg, ga, gb = split_cols(h @ w_in)
    q = rmsnorm(fq.reshape(bsz, seq, FOX_HEADS, FOX_HEAD_DIM), g_q)
    k = rmsnorm(fk.reshape(bsz, seq, FOX_HEADS, FOX_HEAD_DIM), g_k)
    v = fv.reshape(bsz, seq, FOX_HEADS, FOX_HEAD_DIM)
    logf = jax.nn.log_sigmoid((ff + b_fox_f).astype(f32))
    if past is None:
        k_all, v_all, logf_all, offset = k, v, logf, 0
        s0 = jnp.zeros((bsz, HGRN_HEADS, HGRN_EXPAND, HGRN_HEAD_V), f32)
    else:
        pk, pv, plogf, s0 = past
        k_all = jnp.concatenate([pk.astype(k.dtype), k], axis=1)
        v_all = jnp.concatenate([pv.astype(v.dtype), v], axis=1)
        logf_all = jnp.concatenate([plogf.astype(f32), logf], axis=1)
        offset = pk.shape[1]
    fcum = jnp.cumsum(logf_all, axis=1)
    y_a = fox_attention(q, k_all, v_all, fcum[:, offset:], fcum, offset).reshape(bsz, seq, FOX_WIDTH)
    fgate = lb + (1.0 - lb) * jax.nn.sigmoid(hf.astype(f32))
    kshape = (bsz, seq, HGRN_HEADS, HGRN_EXPAND)
    vshape = (bsz, seq, HGRN_HEADS, HGRN_HEAD_V)
    o, s_fin = hgrn2_recurrence(jax.nn.silu(hq).reshape(kshape), (1.0 - fgate).reshape(kshape),
                                hi.reshape(vshape), jnp.log(fgate).reshape(kshape), s0)
    o = rmsnorm(o.astype(x.dtype), g_hgrn_o) * jax.nn.sigmoid(hg).reshape(vshape)
    y_b = o.reshape(bsz, seq, HGRN_WIDTH)
    merged = jax.nn.sigmoid(ga) * (y_a @ w_proj_a) + jax.nn.sigmoid(gb) * (y_b @ w_proj_b)
    x = x + gate1 * (merged @ w_out)
    h2 = rmsnorm(x, g_norm2) * (1 + scale2) + shift2
    x = x + gate2 * moe_ffn(h2, w_router, b_router, w_exp_gate, w_exp_up, w_exp_down,
                            w_sh_gate, w_sh_up, w_sh_down)
    return x, k, v, logf.astype(x.dtype), s_fin


def setup_inputs(seed: int = 0) -> dict:
    key = jax.random.key(seed)
    ks = jax.random.split(key, 32)
    D = D_MODEL

    def nrm(k, shape, s):
        return jax.random.normal(k, shape, jnp.float32) * s

    return {
        'x_prompt': nrm(ks[0], (BATCH, SEQ, D), 1.0),
        'x_sample': nrm(ks[1], (DEC_BATCH, DEC_SEQ, D), 1.0),
        'cache_fox_k': nrm(ks[2], (DEPTH, DEC_BATCH, PAST_LEN, FOX_HEADS, FOX_HEAD_DIM), 1.0),
        'cache_fox_v': nrm(ks[3], (DEPTH, DEC_BATCH, PAST_LEN, FOX_HEADS, FOX_HEAD_DIM), 1.0),
        'cache_fox_logf': jax.nn.log_sigmoid(3.0 + nrm(ks[4], (DEPTH, DEC_BATCH, PAST_LEN, FOX_HEADS), 1.0)),
        'state_hgrn': nrm(ks[5], (DEPTH, DEC_BATCH, HGRN_HEADS, HGRN_EXPAND, HGRN_HEAD_V), 0.5),
        'c_prompt': nrm(ks[6], (BATCH, D), 1.0),
        'c_sample': nrm(ks[7], (DEC_BATCH, D), 1.0),
        'w_ada': nrm(ks[8], (DEPTH, D, 6 * D), 0.5 * D ** -0.5),
        'b_ada': nrm(ks[9], (DEPTH, 6 * D), 0.02),
        'g_norm1': 1.0 + nrm(ks[10], (DEPTH, D), 0.05),
        'w_in': nrm(ks[11], (DEPTH, D, IN_COLS), D ** -0.5),
        'b_fox_f': 3.0 + nrm(ks[12], (DEPTH, FOX_HEADS), 0.5),
        'g_q': 1.0 + nrm(ks[13], (DEPTH, FOX_HEAD_DIM), 0.05),
        'g_k': 1.0 + nrm(ks[14], (DEPTH, FOX_HEAD_DIM), 0.05),
        'hgrn_lb': nrm(ks[15], (DEPTH + 1, HGRN_WIDTH), 0.5),
        'g_hgrn_o': 1.0 + nrm(ks[16], (DEPTH, HGRN_HEAD_V), 0.05),
        'w_proj_a': nrm(ks[17], (DEPTH, FOX_WIDTH, D), FOX_WIDTH ** -0.5),
        'w_proj_b': nrm(ks[18], (DEPTH, HGRN_WIDTH, D), HGRN_WIDTH ** -0.5),
        'w_out': nrm(ks[19], (DEPTH, D, D), D ** -0.5),
        'g_norm2': 1.0 + nrm(ks[20], (DEPTH, D), 0.05),
        'w_router': nrm(ks[21], (DEPTH, D, N_EXPERTS), D ** -0.5),
        'b_router': nrm(ks[22], (DEPTH, N_EXPERTS), 0.01),
        'w_exp_gate': nrm(ks[23], (DEPTH, N_EXPERTS, D, D_EXPERT), D ** -0.5),
        'w_exp_up': nrm(ks[24], (DEPTH, N_EXPERTS, D, D_EXPERT), D ** -0.5),
        'w_exp_down': nrm(ks[25], (DEPTH, N_EXPERTS, D_EXPERT, D), D_EXPERT ** -0.5),
        'w_sh_gate': nrm(ks[26], (DEPTH, D, D_SHARED), D ** -0.5),
        'w_sh_up': nrm(ks[27], (DEPTH, D, D_SHARED), D ** -0.5),
        'w_sh_down': nrm(ks[28], (DEPTH, D_SHARED, D), D_SHARED ** -0.5),
    }


def reference(x_prompt, x_sample, cache_fox_k, cache_fox_v, cache_fox_logf, state_hgrn, c_prompt, c_sample,
              w_ada, b_ada, g_norm1, w_in, b_fox_f, g_q, g_k, hgrn_lb, g_hgrn_o, w_proj_a, w_proj_b, w_out,
              g_norm2, w_router, b_router, w_exp_gate, w_exp_up, w_exp_down, w_sh_gate, w_sh_up, w_sh_down):
    lb_all = jnp.cumsum(jax.nn.softmax(hgrn_lb.astype(jnp.float32), axis=0), axis=0)
    xp, xs = x_prompt, x_sample
    kp_l, vp_l, fp_l, sp_l = [], [], [], []
    ks_l, vs_l, fs_l, ss_l = [], [], [], []
    for l in range(DEPTH):
        lw = (w_ada[l], b_ada[l], g_norm1[l], w_in[l], b_fox_f[l], g_q[l], g_k[l], g_hgrn_o[l],
              w_proj_a[l], w_proj_b[l], w_out[l], g_norm2[l], w_router[l], b_router[l],
              w_exp_gate[l], w_exp_up[l], w_exp_down[l], w_sh_gate[l], w_sh_up[l], w_sh_down[l])
        xp, kp, vp, fp, sp = trunk_layer(xp, c_prompt, None, lb_all[l], *lw)
        past = (cache_fox_k[l], cache_fox_v[l], cache_fox_logf[l], state_hgrn[l])
        xs, kn, vn, fn, sn = trunk_layer(xs, c_sample, past, lb_all[l], *lw)
        kp_l.append(kp); vp_l.append(vp); fp_l.append(fp); sp_l.append(sp)
        ks_l.append(kn); vs_l.append(vn); fs_l.append(fn); ss_l.append(sn)
    new_fox_k_prompt = jnp.stack(kp_l, axis=0)
    new_fox_v_prompt = jnp.stack(vp_l, axis=0)
    new_fox_logf_prompt = jnp.stack(fp_l, axis=0)
    new_hgrn_prompt = jnp.stack(sp_l, axis=0)
    new_fox_k_sample = jnp.stack(ks_l, axis=0)
    new_fox_v_sample = jnp.stack(vs_l, axis=0)
    new_fox_logf_sample = jnp.stack(fs_l, axis=0)
    new_hgrn_sample = jnp.stack(ss_l, axis=0)
    return (xp, xs, new_fox_k_prompt, new_fox_v_prompt, new_fox_logf_prompt, new_hgrn_prompt,
            new_fox_k_sample, new_fox_v_sample, new_fox_logf_sample, new_hgrn_sample)
```

```python
import contextlib
import numpy as np
import concourse.bass as bass
import concourse.mybir as mybir
from concourse.bass_utils import run_bass_kernel_spmd

F32 = mybir.dt.float32
BF16 = mybir.dt.bfloat16
AF = mybir.ActivationFunctionType
ALU = mybir.AluOpType
AX = mybir.AxisListType

D = 1024
NE = 256
EPS = 1e-6
C_FQ, C_FK, C_FV, C_FF, C_HQ, C_HF, C_HI, C_HG, C_GA, C_GB = 0, 512, 1024, 1536, 1544, 2056, 2568, 3080, 3592, 4616


class Buf:
    __slots__ = ("name", "w", "r")

    def __init__(self, name=""):
        self.name = name
        self.w = None
        self.r = []


class KB:
    NDMA = 40

    def __init__(self, nc, stack):
        self.nc = nc
        self.stack = stack
        self.E = dict(pe=nc.tensor, act=nc.scalar, dve=nc.vector, pool=nc.gpsimd, sp=nc.sync)
        self.sems = {}
        self.cnt = {}
        for e in self.E:
            self.sems[e] = stack.enter_context(nc.semaphore("prog_" + e))
            self.cnt[e] = 0
        for i in range(self.NDMA):
            k = "dma%d" % i
            self.sems[k] = stack.enter_context(nc.semaphore(k))
            self.cnt[k] = 0
        self.rr = 0
        self.waited = {e: {} for e in self.E}
        self.n_inst = 0

    def sb(self, name, shape, dt):
        return self.stack.enter_context(self.nc.sbuf_tensor(name, list(shape), dt))

    def ps(self, name, shape, dt=F32):
        return self.stack.enter_context(self.nc.psum_tensor(name, list(shape), dt))

    def _wait(self, e, ev):
        if ev is None:
            return
        key, v = ev
        if self.waited[e].get(key, 0) >= v:
            return
        if key == e and e == "pe":
            return
        self.E[e].wait_ge(self.sems[key], v)
        self.waited[e][key] = v

    def _deps(self, e, R, W):
        for b in R:
            self._wait(e, b.w)
        for b in W:
            self._wait(e, b.w)
            for ev in b.r:
                self._wait(e, ev)

    def _commit(self, ev, R, W):
        for b in W:
            b.w = ev
            b.r = []
        for b in R:
            if b.w is not ev:
                b.r.append(ev)
                if len(b.r) > 10:
                    last = {}
                    for kk, v in b.r:
                        if last.get(kk, 0) < v:
                            last[kk] = v
                    b.r = list(last.items())

    def op(self, e, fn, R=(), W=()):
        self._deps(e, R, W)
        ins = fn(self.E[e])
        self.cnt[e] += 1
        ins.then_inc(self.sems[e], 1)
        ev = (e, self.cnt[e])
        self._commit(ev, R, W)
        self.n_inst += 1
        return ev

    def dma(self, q, out, in_, R=(), W=()):
        self._deps(q, R, W)
        k = "dma%d" % self.rr
        self.rr = (self.rr + 1) % self.NDMA
        if self.cnt[k] > 0:
            self._wait(q, (k, self.cnt[k]))
        ins = self.E[q].dma_start(out=out, in_=in_)
        self.cnt[k] += 16
        ins.then_inc(self.sems[k], 16)
        ev = (k, self.cnt[k])
        self._commit(ev, R, W)
        self.n_inst += 1
        return ev

    def barrier(self):
        keys = list(self.sems.keys())
        for e in self.E:
            for kk in keys:
                if kk != e and self.cnt[kk] > 0:
                    self._wait(e, (kk, self.cnt[kk]))

    def finish(self, bufs, e="sp"):
        for b in bufs:
            self._wait(e, b.w)


import os
STOP = int(os.environ.get('KSTOP', '99'))
QZENG = os.environ.get('KQZENG', 'dve')


def build(NPOS, NSMP=2, PAST=1024, moe=True):
    NTA = NPOS * 2
    NOWN = NPOS
    NCT = PAST // 128
    NTK = max(NTA, NCT + 1)
    NMT = NOWN + NSMP
    nc = bass.Bass("TRN2", target_bir_lowering=False)

    def din(name, shape, dt=F32):
        return nc.dram_tensor(name, list(shape), dt, kind="ExternalInput").ap()

    def dout(name, shape, dt=F32):
        return nc.dram_tensor(name, list(shape), dt, kind="ExternalOutput").ap()

    xa = din("xa", [NTA * 128, D])
    xs_in = din("xs", [NSMP * 128, D])
    tmask_in = din("tmask", [128, NTA + NSMP])
    ck = din("ck", [NSMP, PAST, 512])
    cv = din("cv", [NSMP, PAST, 512])
    clf = din("clf", [NSMP, PAST, 8])
    s0 = din("s0", [NSMP, 4, 128, 128])
    cT = din("cT", [128, 8, 1 + NSMP])
    w_ada = din("w_ada", [D, 6 * D])
    b_ada3 = din("b_ada3", [1 + NSMP, 6 * D])
    g13 = din("g13", [1 + NSMP, D])
    g23 = din("g23", [1 + NSMP, D])
    w_in = din("w_in", [D, 5640])
    bffr = din("bffr", [128, 8])
    gqr = din("gqr", [128, 64])
    gkr = din("gkr", [128, 64])
    lbr = din("lbr", [128, 2, 512])
    gor = din("gor", [128, 128])
    w_pa = din("w_pa", [512, D])
    w_pb = din("w_pb", [512, D])
    w_out = din("w_out", [D, D])
    w_rt = din("w_rt", [D, NE])
    brr = din("brr", [128, NE])
    if moe:
        w_eg = din("w_eg", [NE, D, 256])
        w_eu = din("w_eu", [NE, D, 256])
        w_ed = din("w_ed", [NE, 256, D])
    w_sg = din("w_sg", [D, 256])
    w_su = din("w_su", [D, 256])
    w_sd = din("w_sd", [256, D])

    y_out = dout("y", [NMT * 128, D])
    kn_out = dout("kn", [NMT * 128, 512])
    vn_out = dout("vn", [NMT * 128, 512])
    lf_out = dout("lf", [NMT * 128, 8])
    hs_out = dout("hs", [1 + NSMP, 4, 128, 128])
    x1d = nc.dram_tensor("x1d", [NMT * 128, D], F32, kind="Internal").ap()

    with contextlib.ExitStack() as st:
        k = KB(nc, st)
        outbufs = []

        identb = k.sb("identb", [128, 128], BF16)
        identf = k.sb("identf", [128, 128], F32)
        U = k.sb("U", [128, 128], F32)
        ONES = k.sb("ONES", [128, 128], F32)
        LC = k.sb("LC", [128, 128], F32)
        LB1 = k.sb("LB1", [128, 128], F32)
        MT4 = k.sb("MT4", [128, 4, 64], F32)
        TRIB = k.sb("TRIB", [128, 128], BF16)
        SEL3 = k.sb("SEL3", [4, 4, 128], F32)
        SC2 = k.sb("SC2", [128, 2], F32)
        bC = Buf("const")
        k.op("pool", lambda e: e.memset(identf[:], 0.0), W=[bC])
        k.op("pool", lambda e: e.affine_select(out=identf[:], in_=identf[:], pattern=[[-1, 128]], compare_op=ALU.not_equal, fill=1.0, base=0, channel_multiplier=1), R=[bC], W=[bC])
        k.op("pool", lambda e: e.tensor_copy(out=identb[:], in_=identf[:]), R=[bC], W=[bC])
        k.op("pool", lambda e: e.memset(ONES[:], 1.0), W=[bC])
        k.op("pool", lambda e: e.memset(LB1[:], 0.0), W=[bC])
        k.op("pool", lambda e: e.memset(LB1[0:64, 0:64], 1.0), W=[bC])
        k.op("pool", lambda e: e.memset(LB1[64:128, 64:128], 1.0), W=[bC])
        k.op("pool", lambda e: e.affine_select(out=U[:], in_=ONES[:], pattern=[[1, 128]], compare_op=ALU.is_ge, fill=0.0, base=0, channel_multiplier=-1), R=[bC], W=[bC])
        k.op("pool", lambda e: e.tensor_tensor(out=LC[:], in0=U[:], in1=LB1[:], op=ALU.mult), R=[bC], W=[bC])
        k.op("pool", lambda e: e.tensor_copy(out=TRIB[:], in_=U[:]), R=[bC], W=[bC])
        LC4 = k.sb("LC4", [128, 512], F32)
        for h in range(4):
            k.op("pool", lambda e: e.tensor_copy(out=LC4[:, h * 128:(h + 1) * 128], in_=LC[:]), R=[bC], W=[bC])
        for h in range(4):
            k.op("pool", lambda e: e.tensor_copy(out=MT4[0:64, h, :], in_=U[0:64, 0:64]), R=[bC], W=[bC])
            k.op("pool", lambda e: e.tensor_copy(out=MT4[64:128, h, :], in_=U[64:128, 64:128]), R=[bC], W=[bC])
        ZL = k.sb("ZL", [128, 128], BF16)
        ZR = k.sb("ZR", [128, 512], BF16)
        k.op("pool", lambda e: e.memset(ZL[:], 0.0), W=[bC])
        k.op("pool", lambda e: e.memset(ZR[:], 0.0), W=[bC])
        k.op("pool", lambda e: e.memset(SC2[:], 0.0), W=[bC])
        k.op("pool", lambda e: e.memset(SC2[0:64, 0:1], 1.0), W=[bC])
        k.op("pool", lambda e: e.memset(SC2[64:128, 1:2], 1.0), W=[bC])
        k.op("pool", lambda e: e.memset(SEL3[:], 0.0), W=[bC])
        k.op("pool", lambda e: e.affine_select(out=SEL3[:], in_=SEL3[:], pattern=[[-1, 4], [0, 128]], compare_op=ALU.not_equal, fill=1.0, base=0, channel_multiplier=1), R=[bC], W=[bC])

        BFF = k.sb("BFF", [128, 8], F32)
        GQ = k.sb("GQ", [128, 64], F32)
        GK = k.sb("GK", [128, 64], F32)
        LBR = k.sb("LBR", [128, 2, 512], F32)
        OML = k.sb("OML", [128, 512], F32)
        GO = k.sb("GO", [128, 128], F32)
        TM = k.sb("TM", [128, NTA + NSMP], F32)
        KM = k.sb("KM", [128, NTA + NSMP], F32)
        bP = Buf("params")
        k.dma("sp", BFF[:], bffr, W=[bP])
        k.dma("sp", GQ[:], gqr, W=[bP])
        k.dma("sp", GK[:], gkr, W=[bP])
        k.dma("sp", LBR[:], lbr, W=[bP])
        k.dma("sp", GO[:], gor, W=[bP])
        k.dma("sp", TM[:], tmask_in, W=[bP])
        k.op("dve", lambda e: e.tensor_tensor(out=LBR[:, 0, :], in0=LBR[:, 1, :], in1=LBR[:, 0, :], op=ALU.subtract), R=[bP], W=[bP])
        k.op("act", lambda e: e.activation(out=OML[:], in_=LBR[:, 0, :], func=AF.Sigmoid), R=[bP], W=[bP])
        k.op("dve", lambda e: e.tensor_scalar(out=KM[:], in0=TM[:], scalar1=-1.0, scalar2=30000.0, op0=ALU.add, op1=ALU.mult), R=[bP], W=[bP])

        PS = [k.ps("ps%d" % i, [128, 512], F32) for i in range(7)]
        modd = nc.dram_tensor("modd", [1 + NSMP, 6 * D], F32, kind="Internal").ap()
        ktd = nc.dram_tensor("ktd", [NTK, 128, 4, 128], BF16, kind="Internal").ap()
        vad = nc.dram_tensor("vad", [NTK, 128, 8, 128], BF16, kind="Internal").ap()
        bX1 = [Buf() for _ in range(NMT)]
        bPS = [Buf("ps%d" % i) for i in range(7)]
        PB = k.ps("psb", [128, 1024], BF16)
        bPB = Buf("psb")

        NM = 1 + NSMP
        with contextlib.ExitStack() as st0:
            k.stack = st0
            MOD = k.sb("MOD", [NM, 6 * D], F32)
            bMOD = Buf("MOD")
            CT = k.sb("CT", [128, 8, NM], F32)
            bCT = Buf()
            k.dma("sp", CT[:], cT, W=[bCT])
            k.op("act", lambda e: e.activation(out=CT[:], in_=CT[:], func=AF.Silu), R=[bCT], W=[bCT])
            WA = [k.sb("WA%d" % i, [128, 8, 512], F32) for i in range(2)]
            bWA = [Buf(), Buf()]
            B3 = k.sb("B3", [NM, 6 * D], F32)
            G13 = k.sb("G13", [NM, D], F32)
            G23 = k.sb("G23", [NM, D], F32)
            k.dma("sp", B3[:], b_ada3, W=[bMOD])
            k.dma("sp", G13[:], g13, W=[bMOD])
            k.dma("sp", G23[:], g23, W=[bMOD])
            for g in range(12):
                j = g % 2
                k.dma("sp" if j == 0 else "act", WA[j][:], w_ada[:, g * 512:(g + 1) * 512].rearrange("(c p) n -> p c n", p=128), W=[bWA[j]])
                for c in range(8):
                    k.op("pe", lambda e: e.matmul(PS[j][0:NM, :], lhsT=CT[:, c, :], rhs=WA[j][:, c, :], start=(c == 0), stop=(c == 7)), R=[bCT, bWA[j]], W=[bPS[j]])
                k.op("dve", lambda e: e.tensor_tensor(out=MOD[:, g * 512:(g + 1) * 512], in0=PS[j][0:NM, :], in1=B3[:, g * 512:(g + 1) * 512], op=ALU.add), R=[bPS[j], bMOD], W=[bMOD])
            k.op("dve", lambda e: e.scalar_tensor_tensor(out=MOD[:, D:2 * D], in0=MOD[:, D:2 * D], scalar=1.0, in1=G13[:], op0=ALU.add, op1=ALU.mult), R=[bMOD], W=[bMOD])
            k.op("dve", lambda e: e.scalar_tensor_tensor(out=MOD[:, 4 * D:5 * D], in0=MOD[:, 4 * D:5 * D], scalar=1.0, in1=G23[:], op0=ALU.add, op1=ALU.mult), R=[bMOD], W=[bMOD])
            bMODD = Buf("modd")
            k.dma("sp", modd[:, :], MOD[:], R=[bMOD], W=[bMODD])
            k.barrier()
        k.stack = st

        MB = [k.sb("MB%d" % i, [128, D], F32) for i in range(3)]
        bMB = [Buf() for _ in range(3)]

        def load_mod(m, rows):
            for i, row in enumerate(rows):
                k.dma("sp", MB[i][:], modd[m:m + 1, row * D:(row + 1) * D].partition_broadcast(128), R=[bMODD], W=[bMB[i]])

        with contextlib.ExitStack() as st1:
            k.stack = st1
            NFKM = k.sb("NFKM", [128, NTK, 8], F32)
            bNF = Buf("NF")
            BIASALL = k.sb("BIASALL", [128, NTK, 8], F32)
            bBIASALL = Buf()
            CTOT = k.sb("CTOT", [128, 8], F32)
            bCTOT = Buf()
            CREF = k.sb("CREF", [128, 8], F32)
            bCREF = Buf()
            S = k.sb("S", [128, 4, 128], F32)
            bS = Buf("S")
            SBF = k.sb("SBF", [128, 4, 128], BF16)
            bSBF = Buf("SBF")
            XT = [k.sb("XT%d" % i, [128, D], F32) for i in range(2)]
            bXT = [Buf(), Buf()]
            HF32 = k.sb("HF32", [128, D], F32)
            bHF32 = Buf()
            HB = k.sb("HB", [128, D], BF16)
            bHB = Buf()
            HT = [k.sb("HT%d" % i, [128, 8, 128], BF16) for i in range(2)]
            bHT = [Buf(), Buf()]
            WB = [k.sb("WB%d" % i, [128, 8, 512], BF16) for i in range(3)]
            bWB = [Buf(), Buf(), Buf()]
            wrr = [0]
            WFF = k.sb("WFF", [128, 8, 8], BF16)
            bWFF = Buf()
            k.dma("pool", WFF[:], w_in[:, C_FF:C_FF + 8].rearrange("(c p) n -> p c n", p=128), W=[bWFF])
            T = [k.sb("T%d" % i, [128, 512], F32) for i in range(4)]
            bT = [Buf("T%d" % i) for i in range(4)]
            SM = k.sb("SM", [128, 64], F32)
            bSM = Buf("SM")
            EB = [k.sb("EB%d" % i, [128, 512], F32) for i in range(2)]
            bEB = [Buf(), Buf()]
            KH = [k.sb("KH%d" % i, [128, 512], BF16) for i in range(2)]
            bKH = [Buf(), Buf()]
            IB = [k.sb("IB%d" % i, [128, 512], BF16) for i in range(2)]
            bIB = [Buf(), Buf()]
            QH = [k.sb("QH%d" % i, [128, 512], BF16) for i in range(2)]
            bQH = [Buf(), Buf()]
            SG = [k.sb("SG%d" % i, [128, 512], BF16) for i in range(2)]
            bSG = [Buf(), Buf()]
            EBL = [k.sb("EBL%d" % i, [128, 8], F32) for i in range(2)]
            bEBL = [Buf(), Buf()]
            QHT = k.sb("QHT", [128, 4, 128], BF16)
            bQHT = Buf()
            QZ = [k.sb("QZ%d" % i, [128, 4, 128], BF16) for i in range(2)]
            bQZ = [Buf(), Buf()]
            KHZ = [[k.sb("KHZ%d%d" % (i, c), [128, 512], BF16) for c in range(2)] for i in range(2)]
            bKHZ = [[Buf(), Buf()], [Buf(), Buf()]]
            for i in range(2):
                k.op("pool", lambda e: e.memset(QZ[i][:], 0.0), W=[bQZ[i]])
                for c in range(2):
                    k.op("pool", lambda e: e.memset(KHZ[i][c][:], 0.0), W=[bKHZ[i][c]])
            KHT = k.sb("KHT", [128, 4, 128], BF16)
            bKHT = Buf()
            AM = k.sb("AM", [128, 4, 128], BF16)
            bAM = Buf()
            YBT = [k.sb("YBT%d" % i, [128, 4, 128], BF16) for i in range(2)]
            bYBT = [Buf(), Buf()]
            QT = k.sb("QT", [128, 4, 256], BF16)
            bQT = Buf()
            YAT = k.sb("YAT", [64, 8, 256], BF16)
            bYAT = Buf()
            PT = [k.sb("PT%d" % i, [128, 256], BF16) for i in range(3)]
            bPT = [Buf() for _ in range(3)]
            RC = k.sb("RC", [128, 256], F32)
            bRC = Buf()
            UAC = [[k.sb("UAC%d%d" % (i, j), [128, 512], F32) for j in range(2)] for i in range(2)]
            bUAC = [[Buf(), Buf()], [Buf(), Buf()]]
            MG = k.sb("MG", [128, D], BF16)
            bMG = Buf()
            MGT = k.sb("MGT", [128, 8, 128], BF16)
            bMGT = Buf()
            KTS = [k.sb("KTS%d" % i, [128, 4, 128], BF16) for i in range(2)]
            bKTS = [Buf(), Buf()]
            VAS = [k.sb("VAS%d" % i, [128, 8, 128], BF16) for i in range(2)]
            bVAS = [Buf(), Buf()]
            KTW = k.sb("KTW", [128, 4, 128], BF16)
            bKTW = Buf()
            VAW = k.sb("VAW", [128, 8, 128], BF16)
            bVAW = Buf()
            bKTD = [Buf() for _ in range(NTK)]
            bVAD = [Buf() for _ in range(NTK)]
            k.op("pool", lambda e: e.memset(VAW[:], 1.0), W=[bVAW])

            def load_w(src_ap, q="pool", parts=128):
                j = wrr[0] % 3
                wrr[0] += 1
                nh = src_ap.shape[1]
                k.dma(q, WB[j][0:parts, 0:nh, :], src_ap, W=[bWB[j]])
                return WB[j], bWB[j]

            def w_in_cols(c0, n=512):
                return w_in[:, c0:c0 + n].rearrange("(c p) n -> p c n", p=128)

            def proj(ti, W_, bW, bank, ncols=512):
                for c in range(8):
                    k.op("pe", lambda e: e.matmul(PS[bank][:, 0:ncols], lhsT=HT[ti][:, c, :], rhs=W_[:, c, 0:ncols], start=(c == 0), stop=(c == 7)), R=[bHT[ti], bW], W=[bPS[bank]])

            def rms_heads(src, bsrc, nh, hd, G, dst, bdst, scr, bscr):
                s3 = src.rearrange("p (h d) -> p h d", h=nh)
                q3 = scr.rearrange("p (h d) -> p h d", h=nh)
                d3 = dst.rearrange("p (h d) -> p h d", h=nh)
                k.op("pool", lambda e: e.tensor_tensor(out=scr, in0=src, in1=src, op=ALU.mult), R=[bsrc], W=[bscr])
                k.op("dve", lambda e: e.tensor_reduce(out=SM[:, 0:nh], in_=q3, axis=AX.X, op=ALU.add), R=[bscr], W=[bSM])
                k.op("dve", lambda e: e.tensor_scalar(out=SM[:, 0:nh], in0=SM[:, 0:nh], scalar1=1.0 / hd, scalar2=EPS, op0=ALU.mult, op1=ALU.add), R=[bSM], W=[bSM])
                k.op("act", lambda e: e.activation(out=SM[:, 0:nh], in_=SM[:, 0:nh], func=AF.Sqrt), R=[bSM], W=[bSM])
                k.op("dve", lambda e: e.reciprocal(out=SM[:, 0:nh], in_=SM[:, 0:nh]), R=[bSM], W=[bSM])
                k.op("dve", lambda e: e.tensor_tensor(out=q3, in0=s3, in1=SM[:, 0:nh].unsqueeze(2).to_broadcast([128, nh, hd]), op=ALU.mult), R=[bsrc, bSM], W=[bscr])
                k.op("dve", lambda e: e.tensor_tensor(out=d3, in0=q3, in1=G.unsqueeze(1).to_broadcast([128, nh, hd]), op=ALU.mult), R=[bscr, bP], W=[bdst])

            def norm_tile(x_ap, xi, ti, bsrc=None):
                k.dma("sp", XT[xi][:], x_ap, R=([bsrc] if bsrc else []), W=[bXT[xi]])
                k.op("act", lambda e: e.activation(out=HF32[:], in_=XT[xi][:], func=AF.Square, accum_out=SM[:, 32:33]), R=[bXT[xi]], W=[bHF32, bSM])
                k.op("dve", lambda e: e.tensor_scalar(out=SM[:, 32:33], in0=SM[:, 32:33], scalar1=1.0 / D, scalar2=EPS, op0=ALU.mult, op1=ALU.add), R=[bSM], W=[bSM])
                k.op("act", lambda e: e.activation(out=SM[:, 32:33], in_=SM[:, 32:33], func=AF.Sqrt), R=[bSM], W=[bSM])
                k.op("dve", lambda e: e.reciprocal(out=SM[:, 32:33], in_=SM[:, 32:33]), R=[bSM], W=[bSM])
                k.op("dve", lambda e: e.scalar_tensor_tensor(out=HF32[:], in0=XT[xi][:], scalar=SM[:, 32:33], in1=MB[0][:], op0=ALU.mult, op1=ALU.mult), R=[bXT[xi], bSM, bMB[0]], W=[bHF32])
                k.op("pool", lambda e: e.tensor_tensor(out=HB[:], in0=HF32[:], in1=MB[1][:], op=ALU.add), R=[bHF32, bMB[1]], W=[bHB])
                for c in range(8):
                    k.op("pe", lambda e: e.transpose(out=PB[:, c * 128:(c + 1) * 128], in_=HB[:, c * 128:(c + 1) * 128], identity=identb[:]), R=[bHB, bC], W=[bPB])
                k.op("act", lambda e: e.copy(out=HT[ti][:].rearrange("p c t -> p (c t)"), in_=PB[:, :]), R=[bPB], W=[bHT[ti]])

            def store_kt(kt):
                for c in range(4):
                    k.op("pe", lambda e: e.transpose(out=PB[:, c * 128:(c + 1) * 128], in_=HB[:, c * 128:(c + 1) * 128], identity=identb[:]), R=[bHB, bC], W=[bPB])
                k.op("dve", lambda e: e.tensor_copy(out=KTW[:].rearrange("p c t -> p (c t)"), in_=PB[:, 0:512]), R=[bPB], W=[bKTW])
                k.dma("act", ktd[kt], KTW[:], R=[bKTW], W=[bKTD[kt]])

            def cum_logf(kt, km_col, first_ref):
                k.op("pe", lambda e: e.matmul(PS[1][:, 0:8], lhsT=U[:], rhs=SM[:, 8:16], start=True, stop=True), R=[bC, bSM], W=[bPS[1]])
                k.op("pe", lambda e: e.matmul(PS[1][:, 8:16], lhsT=ONES[:], rhs=SM[:, 8:16], start=True, stop=True), R=[bC, bSM], W=[bPS[1]])
                k.op("dve", lambda e: e.scalar_tensor_tensor(out=NFKM[:, kt, :], in0=PS[1][:, 0:8], scalar=km_col, in1=CTOT[:], op0=ALU.add, op1=ALU.add), R=[bPS[1], bP, bCTOT], W=[bNF])
                if first_ref == "mid":
                    k.op("dve", lambda e: e.tensor_tensor(out=CREF[:], in0=PS[1][:, 8:16], in1=CTOT[:], op=ALU.add), R=[bPS[1], bCTOT], W=[bCREF])
                elif first_ref == "start":
                    k.op("dve", lambda e: e.tensor_copy(out=CREF[:], in_=CTOT[:]), R=[bCTOT], W=[bCREF])
                k.op("dve", lambda e: e.tensor_tensor(out=CTOT[:], in0=PS[1][:, 8:16], in1=CTOT[:], op=ALU.add), R=[bPS[1], bCTOT], W=[bCTOT])

            def process_position(x_aps, tcols, kt0, own, orow, qw, smp=None):
                nt = len(x_aps)
                for ti in range(nt):
                    norm_tile(x_aps[ti], ti, ti)
                W_, bW = load_w(w_in_cols(C_HF))
                for ti in range(nt):
                    proj(ti, W_, bW, 0)
                    k.op("act", lambda e: e.activation(out=T[0][:], in_=PS[0][:, :], func=AF.Sigmoid, scale=-1.0), R=[bPS[0]], W=[bT[0]])
                    k.op("dve", lambda e: e.tensor_tensor(out=T[0][:], in0=T[0][:], in1=OML[:], op=ALU.mult), R=[bT[0], bP], W=[bT[0]])
                    k.op("pool", lambda e: e.tensor_scalar(out=T[1][:], in0=T[0][:], scalar1=-1.0, scalar2=1.0, op0=ALU.mult, op1=ALU.add), R=[bT[0]], W=[bT[1]])
                    k.op("act", lambda e: e.activation(out=T[1][:], in_=T[1][:], func=AF.Ln), R=[bT[1]], W=[bT[1]])
                    k.op("pe", lambda e: e.matmul(PS[1][:, :], lhsT=LC[:], rhs=T[1][:], start=True, stop=True), R=[bC, bT[1]], W=[bPS[1]])
                    k.op("act", lambda e: e.copy(out=EB[ti][:], in_=PS[1][:, :]), R=[bPS[1]], W=[bEB[ti]])
                    k.op("act", lambda e: e.activation(out=T[2][:], in_=PS[1][:, :], func=AF.Exp, scale=-1.0), R=[bPS[1]], W=[bT[2]])
                    k.op("dve", lambda e: e.tensor_tensor(out=KH[ti][:], in0=T[0][:], in1=T[2][:], op=ALU.mult), R=[bT[0], bT[2]], W=[bKH[ti]])
                    for c in range(2):
                        k.op("dve", lambda e: e.tensor_tensor(out=KHZ[ti][c][c * 64:(c + 1) * 64, :], in0=T[0][c * 64:(c + 1) * 64, :], in1=T[2][c * 64:(c + 1) * 64, :], op=ALU.mult), R=[bT[0], bT[2]], W=[bKHZ[ti][c]])
                    for h in range(4):
                        k.op("pe", lambda e: e.matmul(PS[2][:, h * 2:h * 2 + 2], lhsT=T[1][:, h * 128:(h + 1) * 128], rhs=SC2[:], start=True, stop=True), R=[bT[1], bC], W=[bPS[2]])
                    k.op("act", lambda e: e.activation(out=EBL[ti][:], in_=PS[2][:, 0:8], func=AF.Exp), R=[bPS[2]], W=[bEBL[ti]])
                if STOP == 1: return
                W_, bW = load_w(w_in_cols(C_HI))
                for ti in range(nt):
                    proj(ti, W_, bW, 0)
                    k.op("act", lambda e: e.activation(out=IB[ti][:], in_=PS[0][:, :], func=AF.Copy, scale=TM[:, tcols[ti]:tcols[ti] + 1]), R=[bPS[0], bP], W=[bIB[ti]])
                if STOP == 2: return
                W_, bW = load_w(w_in_cols(C_FK))
                for ti in range(nt):
                    proj(ti, W_, bW, 0)
                    k.op("act", lambda e: e.copy(out=T[0][:], in_=PS[0][:, :]), R=[bPS[0]], W=[bT[0]])
                    rms_heads(T[0][:], bT[0], 8, 64, GK[:], T[2][:], bT[2], T[1][:], bT[1])
                    if own:
                        outbufs.append(Buf())
                        k.dma("sp", kn_out[(orow + ti) * 128:(orow + ti + 1) * 128, :], T[2][:], R=[bT[2]], W=[outbufs[-1]])
                    k.op("act", lambda e: e.copy(out=HB[:, 0:512], in_=T[2][:]), R=[bT[2]], W=[bHB])
                    store_kt(kt0 + ti)
                if STOP == 21: return
                W_, bW = load_w(w_in_cols(C_FV))
                for ti in range(nt):
                    kt = kt0 + ti
                    proj(ti, W_, bW, 0)
                    k.op("dve", lambda e: e.tensor_copy(out=T[3][:], in_=PS[0][:, :]), R=[bPS[0]], W=[bT[3]])
                    k.op("act", lambda e: e.copy(out=VAW[:, :, 0:64], in_=T[3][:].rearrange("p (h d) -> p h d", h=8)), R=[bT[3]], W=[bVAW])
                    k.dma("act", vad[kt], VAW[:], R=[bVAW], W=[bVAD[kt]])
                    if own:
                        outbufs.append(Buf())
                        k.dma("sp", vn_out[(orow + ti) * 128:(orow + ti + 1) * 128, :], T[3][:], R=[bT[3]], W=[outbufs[-1]])
                if STOP == 3: return
                for ti in range(nt):
                    proj(ti, WFF, bWFF, 0, ncols=8)
                    k.op("dve", lambda e: e.tensor_tensor(out=SM[:, 0:8], in0=PS[0][:, 0:8], in1=BFF[:], op=ALU.add), R=[bPS[0], bP], W=[bSM])
                    k.op("act", lambda e: e.activation(out=SM[:, 0:8], in_=SM[:, 0:8], func=AF.Exp, scale=-1.0), R=[bSM], W=[bSM])
                    k.op("act", lambda e: e.activation(out=SM[:, 8:16], in_=SM[:, 0:8], func=AF.Ln, bias=1.0), R=[bSM], W=[bSM])
                    if own:
                        k.op("dve", lambda e: e.tensor_scalar(out=SM[:, 16:24], in0=SM[:, 8:16], scalar1=-1.0, scalar2=None, op0=ALU.mult), R=[bSM], W=[bSM])
                        outbufs.append(Buf())
                        k.dma("sp", lf_out[(orow + ti) * 128:(orow + ti + 1) * 128, :], SM[:, 16:24], R=[bSM], W=[outbufs[-1]])
                    cum_logf(kt0 + ti, KM[:, tcols[ti]:tcols[ti] + 1], ("mid" if nt == 2 else "start") if ti == 0 else None)
                if own:
                    W_, bW = load_w(w_in_cols(C_HQ))
                    for ti in range(nt):
                        proj(ti, W_, bW, 0)
                        k.op("act", lambda e: e.activation(out=T[0][:], in_=PS[0][:, :], func=AF.Silu), R=[bPS[0]], W=[bT[0]])
                        k.op("act", lambda e: e.activation(out=T[1][:], in_=EB[ti][:], func=AF.Exp), R=[bEB[ti]], W=[bT[1]])
                        k.op("dve", lambda e: e.tensor_tensor(out=QH[ti][:], in0=T[0][:], in1=T[1][:], op=ALU.mult), R=[bT[0], bT[1]], W=[bQH[ti]])
                    W_, bW = load_w(w_in_cols(C_HG))
                    for ti in range(nt):
                        proj(ti, W_, bW, 0)
                        k.op("act", lambda e: e.activation(out=SG[ti][:], in_=PS[0][:, :], func=AF.Sigmoid), R=[bPS[0]], W=[bSG[ti]])
                if STOP == 4: return
                for ti in range(nt):
                    if STOP in (51, 52) and not own: return
                    if own and STOP != 55:
                        for h in range(4):
                            k.op("pe", lambda e: e.transpose(out=PB[:, h * 128:(h + 1) * 128], in_=QH[ti][:, h * 128:(h + 1) * 128], identity=identb[:]), R=[bQH[ti], bC], W=[bPB])
                            k.op("pe", lambda e: e.transpose(out=PB[:, 512 + h * 128:512 + (h + 1) * 128], in_=KH[ti][:, h * 128:(h + 1) * 128], identity=identb[:]), R=[bKH[ti], bC], W=[bPB])
                        k.op("act", lambda e: e.copy(out=QHT[:].rearrange("p h t -> p (h t)"), in_=PB[:, 0:512]), R=[bPB], W=[bQHT])
                        if STOP == 511: return
                        for c in range(2):
                            k.op(QZENG, lambda e: e.tensor_copy(out=QZ[c][:, :, c * 64:(c + 1) * 64], in_=QHT[:, :, c * 64:(c + 1) * 64]), R=[bQHT], W=[bQZ[c]])
                        if STOP == 512: return
                        k.op("dve", lambda e: e.tensor_copy(out=KHT[:].rearrange("p h t -> p (h t)"), in_=PB[:, 512:1024]), R=[bPB], W=[bKHT])
                        if STOP == 51: return
                        for h in range(4):
                            k.op("pe", lambda e: e.matmul(PS[3][:, h * 128:(h + 1) * 128], lhsT=KHT[:, h, :], rhs=QHT[:, h, :], start=True, stop=True), R=[bKHT, bQHT], W=[bPS[3]])
                        k.op("dve", lambda e: e.tensor_tensor(out=AM[:].rearrange("p h t -> p (h t)"), in0=PS[3][:, :], in1=LC4[:], op=ALU.mult), R=[bPS[3], bC], W=[bAM])
                        if STOP == 52: return
                        k.op("pe", lambda e: e.matmul(PS[4][:, :], lhsT=ZL[:], rhs=ZR[:], start=True, stop=False), R=[bC], W=[bPS[4]])
                        for h in range(4):
                            k.op("pe", lambda e: e.matmul(PS[4][:, h * 128:(h + 1) * 128], lhsT=AM[:, h, :], rhs=IB[ti][:, h * 128:(h + 1) * 128], start=False, stop=False, skip_group_check=True), R=[bAM, bIB[ti]], W=[bPS[4]])
                            k.op("pe", lambda e: e.matmul(PS[4][:, h * 128:(h + 1) * 128], lhsT=QZ[0][:, h, :], rhs=SBF[:, h, :], start=False, stop=(smp is not None and h == 3), skip_group_check=True), R=[bQZ[0], bSBF], W=[bPS[4]])
                    if STOP == 53: return
                    nch = 1 if smp is not None else 2
                    for c in range(nch):
                        if own and c == 1 and STOP != 55:
                            for h in range(4):
                                k.op("pe", lambda e: e.matmul(PS[4][:, h * 128:(h + 1) * 128], lhsT=QZ[1][:, h, :], rhs=SBF[:, h, :], start=False, stop=(h == 3), skip_group_check=True), R=[bQZ[1], bSBF], W=[bPS[4]])
                        for h in range(4):
                            k.op("pe", lambda e: e.matmul(PS[5][:, h * 128:(h + 1) * 128], lhsT=KHZ[ti][c][:, h * 128:(h + 1) * 128], rhs=IB[ti][:, h * 128:(h + 1) * 128], start=True, stop=True), R=[bKHZ[ti][c], bIB[ti]], W=[bPS[5]])
                        k.op("dve", lambda e: e.tensor_tensor(out=S[:].rearrange("p h e -> p (h e)"), in0=PS[5][:, :], in1=S[:].rearrange("p h e -> p (h e)"), op=ALU.add), R=[bPS[5], bS], W=[bS])
                        k.op("dve", lambda e: e.tensor_tensor(out=S[:], in0=S[:], in1=EBL[ti][:].rearrange("p (h c) -> p h c", c=2)[:, :, c:c + 1].to_broadcast([128, 4, 128]), op=ALU.mult), R=[bS, bEBL[ti]], W=[bS])
                        k.op("act", lambda e: e.copy(out=SBF[:], in_=S[:]), R=[bS], W=[bSBF])
                    if STOP == 54: return
                    if STOP == 55: continue
                    if own:
                        k.op("act", lambda e: e.copy(out=T[0][:], in_=PS[4][:, :]), R=[bPS[4]], W=[bT[0]])
                        rms_heads(T[0][:], bT[0], 4, 128, GO[:], T[2][:], bT[2], T[1][:], bT[1])
                        k.op("dve", lambda e: e.tensor_tensor(out=HB[:, 0:512], in0=T[2][:], in1=SG[ti][:], op=ALU.mult), R=[bT[2], bSG[ti]], W=[bHB])
                        for h in range(4):
                            k.op("pe", lambda e: e.transpose(out=PB[:, h * 128:(h + 1) * 128], in_=HB[:, h * 128:(h + 1) * 128], identity=identb[:]), R=[bHB, bC], W=[bPB])
                        k.op("act", lambda e: e.copy(out=YBT[ti][:].rearrange("p h t -> p (h t)"), in_=PB[:, 0:512]), R=[bPB], W=[bYBT[ti]])
                if smp is not None:
                    outbufs.append(Buf())
                    k.dma("sp", hs_out[1 + smp].rearrange("h d e -> d h e"), S[:], R=[bS], W=[outbufs[-1]])
                if not own:
                    return
                if STOP == 5: return
                W_, bW = load_w(w_in_cols(C_FQ))
                for ti in range(nt):
                    proj(ti, W_, bW, 0)
                    k.op("act", lambda e: e.copy(out=T[0][:], in_=PS[0][:, :]), R=[bPS[0]], W=[bT[0]])
                    rms_heads(T[0][:], bT[0], 8, 64, GQ[:], T[2][:], bT[2], T[1][:], bT[1])
                    k.op("act", lambda e: e.copy(out=HB[:, 0:512], in_=T[2][:]), R=[bT[2]], W=[bHB])
                    for c in range(4):
                        k.op("pe", lambda e: e.transpose(out=PB[:, c * 128:(c + 1) * 128], in_=HB[:, c * 128:(c + 1) * 128], identity=identb[:]), R=[bHB, bC], W=[bPB])
                    k.op("dve", lambda e: e.tensor_copy(out=QT[:, :, ti * 128:(ti + 1) * 128], in_=PB[:, 0:512].rearrange("p (c t) -> p c t", c=4)), R=[bPB], W=[bQT])
                if STOP == 6: return
                nkt = kt0 + nt
                k.op("dve", lambda e: e.tensor_tensor(out=BIASALL[:, 0:nkt, :], in0=NFKM[:, 0:nkt, :], in1=CREF[:].unsqueeze(1).to_broadcast([128, nkt, 8]), op=ALU.subtract), R=[bNF, bCREF], W=[bBIASALL])
                pi = 0
                for ob in range(4):
                    k.op("pe", lambda e: e.matmul(PS[ob][:, :], lhsT=ZL[:], rhs=ZR[:], start=True, stop=False), R=[bC], W=[bPS[ob]])
                for kt in range(nkt):
                    j = kt % 2
                    k.dma("sp", KTS[j][:], ktd[kt], R=[bKTD[kt]], W=[bKTS[j]])
                    k.dma("sp", VAS[j][:], vad[kt], R=[bVAD[kt]], W=[bVAS[j]])
                    dj = kt - kt0
                    q0 = 0 if dj < 0 else dj * 128
                    nq = qw - q0
                    for h in range(8):
                        hp, ho = h // 2, (h % 2) * 64
                        ob, oc = h // 2, (h % 2) * 256
                        sb_ = 4 + (pi % 2)
                        p_, bp_ = PT[pi % 3], bPT[pi % 3]
                        pi += 1
                        k.op("pe", lambda e: e.matmul(PS[sb_][:, 0:nq], lhsT=KTS[j][ho:ho + 64, hp, :], rhs=QT[ho:ho + 64, hp, q0:qw], start=True, stop=True), R=[bKTS[j], bQT], W=[bPS[sb_]])
                        k.op("act", lambda e: e.activation(out=p_[:, 0:nq], in_=PS[sb_][:, 0:nq], func=AF.Exp, bias=BIASALL[:, kt, h:h + 1], scale=0.125), R=[bPS[sb_], bBIASALL], W=[bp_])
                        if dj >= 0:
                            k.op("dve", lambda e: e.tensor_tensor(out=p_[:, 0:128], in0=p_[:, 0:128], in1=TRIB[:], op=ALU.mult), R=[bp_, bC], W=[bp_])
                        k.op("pe", lambda e: e.matmul(PS[ob][:, oc + q0:oc + qw], lhsT=VAS[j][:, h, :], rhs=p_[:, 0:nq], start=False, stop=(kt == nkt - 1 and h % 2 == 1), skip_group_check=True), R=[bVAS[j], bp_], W=[bPS[ob]])
                for h in range(8):
                    ob, oc = h // 2, (h % 2) * 256
                    k.op("dve", lambda e: e.reciprocal(out=RC[64:128, 0:qw], in_=PS[ob][64:128, oc:oc + qw]), R=[bPS[ob]], W=[bRC])
                    k.op("dve", lambda e: e.tensor_tensor(out=YAT[:, h, 0:qw], in0=PS[ob][0:64, oc:oc + qw], in1=RC[64:128, 0:qw], op=ALU.mult), R=[bPS[ob], bRC], W=[bYAT])
                if STOP == 7: return
                for half in range(2):
                    Wg_, bWg = load_w(w_in_cols(C_GA + half * 512))
                    Wp_, bWp = load_w(w_pa[:, half * 512:(half + 1) * 512].rearrange("(h p) n -> p h n", p=64), parts=64)
                    for ti in range(nt):
                        proj(ti, Wg_, bWg, 0)
                        k.op("act", lambda e: e.activation(out=T[0][:], in_=PS[0][:, :], func=AF.Sigmoid), R=[bPS[0]], W=[bT[0]])
                        for h in range(8):
                            k.op("pe", lambda e: e.matmul(PS[1][:, :], lhsT=YAT[:, h, ti * 128:(ti + 1) * 128], rhs=Wp_[0:64, h, :], start=(h == 0), stop=(h == 7)), R=[bYAT, bWp], W=[bPS[1]])
                        k.op("dve", lambda e: e.tensor_tensor(out=UAC[ti][half][:], in0=PS[1][:, :], in1=T[0][:], op=ALU.mult), R=[bPS[1], bT[0]], W=[bUAC[ti][half]])
                for half in range(2):
                    Wg_, bWg = load_w(w_in_cols(C_GB + half * 512))
                    Wp_, bWp = load_w(w_pb[:, half * 512:(half + 1) * 512].rearrange("(h p) n -> p h n", p=128))
                    for ti in range(nt):
                        proj(ti, Wg_, bWg, 0)
                        k.op("act", lambda e: e.activation(out=T[0][:], in_=PS[0][:, :], func=AF.Sigmoid), R=[bPS[0]], W=[bT[0]])
                        for h in range(4):
                            k.op("pe", lambda e: e.matmul(PS[1][:, :], lhsT=YBT[ti][:, h, :], rhs=Wp_[:, h, :], start=(h == 0), stop=(h == 3)), R=[bYBT[ti], bWp], W=[bPS[1]])
                        k.op("dve", lambda e: e.tensor_tensor(out=T[1][:], in0=PS[1][:, :], in1=T[0][:], op=ALU.mult), R=[bPS[1], bT[0]], W=[bT[1]])
                        k.op("pool", lambda e: e.tensor_tensor(out=UAC[ti][half][:], in0=UAC[ti][half][:], in1=T[1][:], op=ALU.add), R=[bT[1], bUAC[ti][half]], W=[bUAC[ti][half]])
                Wo = [load_w(w_out[:, half * 512:(half + 1) * 512].rearrange("(c p) n -> p c n", p=128)) for half in range(2)]
                for ti in range(nt):
                    for half in range(2):
                        k.op("act", lambda e: e.copy(out=MG[:, half * 512:(half + 1) * 512], in_=UAC[ti][half][:]), R=[bUAC[ti][half]], W=[bMG])
                    for c in range(8):
                        k.op("pe", lambda e: e.transpose(out=PB[:, c * 128:(c + 1) * 128], in_=MG[:, c * 128:(c + 1) * 128], identity=identb[:]), R=[bMG, bC], W=[bPB])
                    k.op("act", lambda e: e.copy(out=MGT[:].rearrange("p c t -> p (c t)"), in_=PB[:, :]), R=[bPB], W=[bMGT])
                    k.dma("sp", XT[ti][:], x_aps[ti], W=[bXT[ti]])
                    for half in range(2):
                        Wo_, bWo = Wo[half]
                        for c in range(8):
                            k.op("pe", lambda e: e.matmul(PS[1][:, :], lhsT=MGT[:, c, :], rhs=Wo_[:, c, :], start=(c == 0), stop=(c == 7)), R=[bMGT, bWo], W=[bPS[1]])
                        k.op("dve", lambda e: e.tensor_tensor(out=T[0][:], in0=PS[1][:, :], in1=MB[2][:, half * 512:(half + 1) * 512], op=ALU.mult), R=[bPS[1], bMB[2]], W=[bT[0]])
                        k.op("pool", lambda e: e.tensor_tensor(out=XT[ti][:, half * 512:(half + 1) * 512], in0=XT[ti][:, half * 512:(half + 1) * 512], in1=T[0][:], op=ALU.add), R=[bT[0], bXT[ti]], W=[bXT[ti]])
                    k.dma("sp", x1d[(orow + ti) * 128:(orow + ti + 1) * 128, :], XT[ti][:], R=[bXT[ti]], W=[bX1[orow + ti]])

            k.op("pool", lambda e: e.memset(S[:], 0.0), W=[bS])
            k.op("pool", lambda e: e.memset(SBF[:], 0.0), W=[bSBF])
            k.op("pool", lambda e: e.memset(CTOT[:], 0.0), W=[bCTOT])
            load_mod(0, [1, 0, 2])
            for p in range(NPOS):
                own = (p % 2 == 1)
                process_position([xa[(2 * p + i) * 128:(2 * p + i + 1) * 128, :] for i in range(2)], [2 * p, 2 * p + 1], 2 * p, own, (p // 2) * 2, 256)
            outbufs.append(Buf())
            k.dma("sp", hs_out[0].rearrange("h d e -> d h e"), S[:], R=[bS], W=[outbufs[-1]])
            for s in range(NSMP):
                load_mod(1 + s, [1, 0, 2])
                k.op("pool", lambda e: e.memset(CTOT[:], 0.0), W=[bCTOT])
                k.dma("sp", S[:], s0[s].rearrange("h d e -> d h e"), W=[bS])
                k.op("act", lambda e: e.copy(out=SBF[:], in_=S[:]), R=[bS], W=[bSBF])
                for ct in range(NCT):
                    k.dma("sp", T[0][:], ck[s, ct * 128:(ct + 1) * 128, :], W=[bT[0]])
                    k.op("act", lambda e: e.copy(out=HB[:, 0:512], in_=T[0][:]), R=[bT[0]], W=[bHB])
                    store_kt(ct)
                    k.dma("sp", T[3][:], cv[s, ct * 128:(ct + 1) * 128, :], W=[bT[3]])
                    k.op("act", lambda e: e.copy(out=VAW[:, :, 0:64], in_=T[3][:].rearrange("p (h d) -> p h d", h=8)), R=[bT[3]], W=[bVAW])
                    k.dma("act", vad[ct], VAW[:], R=[bVAW], W=[bVAD[ct]])
                    k.dma("sp", SM[:, 0:8], clf[s, ct * 128:(ct + 1) * 128, :], W=[bSM])
                    k.op("dve", lambda e: e.tensor_scalar(out=SM[:, 8:16], in0=SM[:, 0:8], scalar1=-1.0, scalar2=None, op0=ALU.mult), R=[bSM], W=[bSM])
                    cum_logf(ct, 0.0, None)
                process_position([xs_in[s * 128:(s + 1) * 128, :]], [NTA + s], NCT, True, NOWN + s, 128, smp=s)
            k.barrier()
        k.stack = st

        if moe:
            GT = 8
            with contextlib.ExitStack() as st2:
                k.stack = st2
                WR = k.sb("WR", [128, 8, NE], BF16)
                bWR = Buf()
                k.dma("pool", WR[:], w_rt.rearrange("(c p) n -> p c n", p=128), W=[bWR])
                BR = k.sb("BR", [128, NE], F32)
                k.dma("sp", BR[:], brr, W=[bP])
                H2T = k.sb("H2T", [128, 8, GT * 128], BF16)
                bH2T = Buf()
                YACC = [k.sb("YACC%d" % i, [128, D], F32) for i in range(GT)]
                bYACC = [Buf() for _ in range(GT)]
                GG = [k.sb("GG%d" % i, [128, NE + 1], F32) for i in range(GT)]
                bGG = [Buf() for _ in range(GT)]
                XT2 = k.sb("XT2", [128, D], F32)
                bXT2 = Buf()
                HF2 = k.sb("HF2", [128, D], F32)
                bHF2 = Buf()
                HB2 = k.sb("HB2", [128, D], BF16)
                bHB2 = Buf()
                R1 = k.sb("R1", [128, NE], F32)
                bR1 = Buf()
                R2 = k.sb("R2", [128, NE], F32)
                bR2 = Buf()
                R3 = k.sb("R3", [128, NE], F32)
                bR3 = Buf()
                SM2 = k.sb("SM2", [128, 64], F32)
                bSM2 = Buf()
                WG = [k.sb("WG%d" % i, [128, 8, 256], BF16) for i in range(2)]
                WU = [k.sb("WU%d" % i, [128, 8, 256], BF16) for i in range(2)]
                WD = [k.sb("WD%d" % i, [128, 2, D], BF16) for i in range(2)]
                bWE = [Buf(), Buf()]
                AS = k.sb("AS", [128, 2, 512], BF16)
                bAS = Buf()
                AA = [k.sb("AA%d" % i, [128, 2, 512], BF16) for i in range(2)]
                bAA = [Buf(), Buf()]
                ngroups = (NMT + GT - 1) // GT
                cur_m = [-1]
                for g in range(ngroups):
                    tiles = list(range(g * GT, min(NMT, (g + 1) * GT)))
                    ntg = len(tiles)
                    for li, t in enumerate(tiles):
                        m = 0 if t < NOWN else 1 + (t - NOWN)
                        if m != cur_m[0]:
                            load_mod(m, [4, 3, 5])
                            cur_m[0] = m
                        k.dma("sp", XT2[:], x1d[t * 128:(t + 1) * 128, :], R=[bX1[t]], W=[bXT2])
                        k.op("act", lambda e: e.activation(out=HF2[:], in_=XT2[:], func=AF.Square, accum_out=SM2[:, 32:33]), R=[bXT2], W=[bHF2, bSM2])
                        k.op("dve", lambda e: e.tensor_scalar(out=SM2[:, 32:33], in0=SM2[:, 32:33], scalar1=1.0 / D, scalar2=EPS, op0=ALU.mult, op1=ALU.add), R=[bSM2], W=[bSM2])
                        k.op("act", lambda e: e.activation(out=SM2[:, 32:33], in_=SM2[:, 32:33], func=AF.Sqrt), R=[bSM2], W=[bSM2])
                        k.op("dve", lambda e: e.reciprocal(out=SM2[:, 32:33], in_=SM2[:, 32:33]), R=[bSM2], W=[bSM2])
                        k.op("dve", lambda e: e.scalar_tensor_tensor(out=HF2[:], in0=XT2[:], scalar=SM2[:, 32:33], in1=MB[0][:], op0=ALU.mult, op1=ALU.mult), R=[bXT2, bSM2, bMB[0]], W=[bHF2])
                        k.op("pool", lambda e: e.tensor_tensor(out=HB2[:], in0=HF2[:], in1=MB[1][:], op=ALU.add), R=[bHF2, bMB[1]], W=[bHB2])
                        for c in range(8):
                            k.op("pe", lambda e: e.transpose(out=PB[:, c * 128:(c + 1) * 128], in_=HB2[:, c * 128:(c + 1) * 128], identity=identb[:]), R=[bHB2, bC], W=[bPB])
                        k.op("act", lambda e: e.copy(out=H2T[:, :, li * 128:(li + 1) * 128], in_=PB[:, :].rearrange("p (c t) -> p c t", c=8)), R=[bPB], W=[bH2T])
                        k.op("pool", lambda e: e.memset(YACC[li][:], 0.0), W=[bYACC[li]])
                        for c in range(8):
                            k.op("pe", lambda e: e.matmul(PS[6][:, 0:NE], lhsT=H2T[:, c, li * 128:(li + 1) * 128], rhs=WR[:, c, :], start=(c == 0), stop=(c == 7)), R=[bH2T, bWR], W=[bPS[6]])
                        k.op("act", lambda e: e.activation(out=R1[:], in_=PS[6][:, 0:NE], func=AF.Sigmoid), R=[bPS[6]], W=[bR1])
                        k.op("dve", lambda e: e.tensor_tensor(out=R2[:], in0=R1[:], in1=BR[:], op=ALU.add), R=[bR1, bP], W=[bR2])
                        r2g = R2[:].rearrange("p (g e) -> p g e", g=8)
                        r3g = R3[:].rearrange("p (g e) -> p g e", g=8)
                        k.op("dve", lambda e: e.tensor_reduce(out=SM2[:, 0:8], in_=r2g, axis=AX.X, op=ALU.max), R=[bR2], W=[bSM2])
                        k.op("dve", lambda e: e.tensor_tensor(out=r3g, in0=r2g, in1=SM2[:, 0:8].unsqueeze(2).to_broadcast([128, 8, 32]), op=ALU.is_equal), R=[bR2, bSM2], W=[bR3])
                        k.op("dve", lambda e: e.scalar_tensor_tensor(out=R3[:], in0=R3[:], scalar=-1.0e4, in1=R2[:], op0=ALU.mult, op1=ALU.add), R=[bR3, bR2], W=[bR3])
                        k.op("dve", lambda e: e.tensor_reduce(out=SM2[:, 8:16], in_=r3g, axis=AX.X, op=ALU.max), R=[bR3], W=[bSM2])
                        k.op("dve", lambda e: e.tensor_tensor(out=SM2[:, 0:8], in0=SM2[:, 0:8], in1=SM2[:, 8:16], op=ALU.add), R=[bSM2], W=[bSM2])
                        k.op("dve", lambda e: e.max(out=SM2[:, 16:24], in_=SM2[:, 0:8]), R=[bSM2], W=[bSM2])
                        k.op("dve", lambda e: e.tensor_scalar(out=SM2[:, 0:8], in0=SM2[:, 0:8], scalar1=SM2[:, 19:20], scalar2=None, op0=ALU.is_ge), R=[bSM2], W=[bSM2])
                        k.op("dve", lambda e: e.tensor_scalar(out=SM2[:, 0:8], in0=SM2[:, 0:8], scalar1=-1.0, scalar2=1.0e4, op0=ALU.add, op1=ALU.mult), R=[bSM2], W=[bSM2])
                        k.op("dve", lambda e: e.tensor_tensor(out=r3g, in0=r2g, in1=SM2[:, 0:8].unsqueeze(2).to_broadcast([128, 8, 32]), op=ALU.add), R=[bR2, bSM2], W=[bR3])
                        k.op("dve", lambda e: e.max(out=SM2[:, 24:32], in_=R3[:]), R=[bR3], W=[bSM2])
                        k.op("dve", lambda e: e.tensor_scalar(out=R3[:], in0=R3[:], scalar1=SM2[:, 31:32], scalar2=None, op0=ALU.is_ge), R=[bR3, bSM2], W=[bR3])
                        k.op("dve", lambda e: e.tensor_tensor(out=R3[:], in0=R3[:], in1=R1[:], op=ALU.mult), R=[bR3, bR1], W=[bR3])
                        k.op("dve", lambda e: e.tensor_reduce(out=SM2[:, 40:41], in_=R3[:], axis=AX.X, op=ALU.add), R=[bR3], W=[bSM2])
                        k.op("dve", lambda e: e.reciprocal(out=SM2[:, 40:41], in_=SM2[:, 40:41]), R=[bSM2], W=[bSM2])
                        k.op("dve", lambda e: e.tensor_scalar(out=GG[li][:, 0:NE], in0=R3[:], scalar1=SM2[:, 40:41], scalar2=2.5, op0=ALU.mult, op1=ALU.mult), R=[bR3, bSM2], W=[bGG[li]])
                        k.op("pool", lambda e: e.memset(GG[li][:, NE:NE + 1], 1.0), W=[bGG[li]])
                    ntok = ntg * 128
                    chunks = [(c0, min(512, ntok - c0)) for c0 in range(0, ntok, 512)]
                    for ex in range(NE + 1):
                        j = ex % 2
                        if ex < NE:
                            sg_, su_, sd_ = w_eg[ex], w_eu[ex], w_ed[ex]
                        else:
                            sg_, su_, sd_ = w_sg, w_su, w_sd
                        k.dma("pool", WG[j][:], sg_.rearrange("(c p) n -> p c n", p=128), W=[bWE[j]])
                        k.dma("pool", WU[j][:], su_.rearrange("(c p) n -> p c n", p=128), W=[bWE[j]])
                        k.dma("pool", WD[j][:], sd_.rearrange("(c p) n -> p c n", p=128), W=[bWE[j]])
                        for ci, (c0, cn) in enumerate(chunks):
                            for fc in range(2):
                                for c in range(8):
                                    k.op("pe", lambda e: e.matmul(PS[fc][:, 0:cn], lhsT=WG[j][:, c, fc * 128:(fc + 1) * 128], rhs=H2T[:, c, c0:c0 + cn], start=(c == 0), stop=(c == 7)), R=[bWE[j], bH2T], W=[bPS[fc]])
                                for c in range(8):
                                    k.op("pe", lambda e: e.matmul(PS[2 + fc][:, 0:cn], lhsT=WU[j][:, c, fc * 128:(fc + 1) * 128], rhs=H2T[:, c, c0:c0 + cn], start=(c == 0), stop=(c == 7)), R=[bWE[j], bH2T], W=[bPS[2 + fc]])
                            aj = ci % 2
                            for fc in range(2):
                                k.op("act", lambda e: e.activation(out=AS[:, fc, 0:cn], in_=PS[fc][:, 0:cn], func=AF.Silu), R=[bPS[fc]], W=[bAS])
                                k.op("dve", lambda e: e.tensor_tensor(out=AA[aj][:, fc, 0:cn], in0=PS[2 + fc][:, 0:cn], in1=AS[:, fc, 0:cn], op=ALU.mult), R=[bPS[2 + fc], bAS], W=[bAA[aj]])
                            for tt in range(cn // 128):
                                li = (c0 // 128) + tt
                                for hf in range(2):
                                    for fc in range(2):
                                        k.op("pe", lambda e: e.matmul(PS[4 + hf][:, :], lhsT=AA[aj][:, fc, tt * 128:(tt + 1) * 128], rhs=WD[j][:, fc, hf * 512:(hf + 1) * 512], start=(fc == 0), stop=(fc == 1)), R=[bAA[aj], bWE[j]], W=[bPS[4 + hf]])
                                    k.op("dve", lambda e: e.scalar_tensor_tensor(out=YACC[li][:, hf * 512:(hf + 1) * 512], in0=PS[4 + hf][:, :], scalar=GG[li][:, ex:ex + 1], in1=YACC[li][:, hf * 512:(hf + 1) * 512], op0=ALU.mult, op1=ALU.add), R=[bPS[4 + hf], bGG[li], bYACC[li]], W=[bYACC[li]])
                    for li, t in enumerate(tiles):
                        m = 0 if t < NOWN else 1 + (t - NOWN)
                        if m != cur_m[0]:
                            load_mod(m, [4, 3, 5])
                            cur_m[0] = m
                        k.dma("sp", XT2[:], x1d[t * 128:(t + 1) * 128, :], R=[bX1[t]], W=[bXT2])
                        k.op("dve", lambda e: e.tensor_tensor(out=YACC[li][:], in0=YACC[li][:], in1=MB[2][:], op=ALU.mult), R=[bYACC[li], bMB[2]], W=[bYACC[li]])
                        k.op("pool", lambda e: e.tensor_tensor(out=YACC[li][:], in0=YACC[li][:], in1=XT2[:], op=ALU.add), R=[bYACC[li], bXT2], W=[bYACC[li]])
                        outbufs.append(Buf())
                        k.dma("sp", y_out[t * 128:(t + 1) * 128, :], YACC[li][:], R=[bYACC[li]], W=[outbufs[-1]])
                k.barrier()
            k.stack = st
        if not moe:
            for t in range(NMT):
                outbufs.append(Buf())
                k.dma("sp", y_out[t * 128:(t + 1) * 128, :], x1d[t * 128:(t + 1) * 128, :], R=[bX1[t]], W=[outbufs[-1]])
        k.finish(outbufs, "sp")
        k.barrier()
    return nc


_NC_CACHE = {}


def _rep(a, n=128):
    return np.ascontiguousarray(np.broadcast_to(np.asarray(a, np.float32)[None], (n,) + tuple(a.shape)))


def kernel(x_prompt, x_sample, cache_fox_k, cache_fox_v, cache_fox_logf, state_hgrn, c_prompt, c_sample,
           w_ada, b_ada, g_norm1, w_in, b_fox_f, g_q, g_k, hgrn_lb, g_hgrn_o, w_proj_a, w_proj_b, w_out,
           g_norm2, w_router, b_router, w_exp_gate, w_exp_up, w_exp_down, w_sh_gate, w_sh_up, w_sh_down, _moe=True):
    f = lambda a: np.ascontiguousarray(np.asarray(a, dtype=np.float32))
    x_prompt, x_sample = f(x_prompt), f(x_sample)
    B, SEQ, _ = x_prompt.shape
    SB, SS, _ = x_sample.shape
    PAST = cache_fox_k.shape[2]
    NPOS = SEQ // 256
    NSMP = SB // 8
    NTA, NOWN, NMT = NPOS * 2, NPOS, NPOS + NSMP
    key = (NPOS, NSMP, PAST, _moe)
    if key not in _NC_CACHE:
        _NC_CACHE[key] = build(NPOS, NSMP, PAST, moe=_moe)
    nc = _NC_CACHE[key]
    ck = f(cache_fox_k)[0].reshape(SB, PAST, 512)
    cv = f(cache_fox_v)[0].reshape(SB, PAST, 512)
    clf = f(cache_fox_logf)[0]
    st = f(state_hgrn)[0]
    shared = dict(
        w_ada=f(w_ada)[0], w_in=f(w_in)[0], bffr=_rep(f(b_fox_f)[0]), gqr=_rep(f(g_q)[0]), gkr=_rep(f(g_k)[0]),
        lbr=_rep(f(hgrn_lb)), gor=_rep(f(g_hgrn_o)[0]), w_pa=f(w_proj_a)[0], w_pb=f(w_proj_b)[0], w_out=f(w_out)[0],
        w_rt=f(w_router)[0], brr=_rep(f(b_router)[0]), w_eg=f(w_exp_gate)[0], w_eu=f(w_exp_up)[0], w_ed=f(w_exp_down)[0],
        w_sg=f(w_sh_gate)[0], w_su=f(w_sh_up)[0], w_sd=f(w_sh_down)[0],
        b_ada3=_rep(f(b_ada)[0], 1 + NSMP), g13=_rep(f(g_norm1)[0], 1 + NSMP), g23=_rep(f(g_norm2)[0], 1 + NSMP))
    if not _moe:
        for nm in ("w_eg", "w_eu", "w_ed"):
            shared.pop(nm)
    in_maps = []
    for c in range(8):
        b, j = c // 2, c % 2
        if j == 1:
            xa = x_prompt[b]
        else:
            xa = np.concatenate([np.zeros((256, D), np.float32), x_prompt[b][:SEQ - 256]], axis=0)
        tm = np.ones((128, NTA + NSMP), np.float32)
        if j == 0:
            tm[:, 0:2] = 0.0
        tm[SS:, NTA:] = 0.0
        xs = np.zeros((NSMP, 128, D), np.float32)
        sidx = [c * NSMP + s for s in range(NSMP)]
        for s, si in enumerate(sidx):
            xs[s, :SS] = x_sample[si]
        cvecs = np.stack([f(c_prompt)[b]] + [f(c_sample)[si] for si in sidx], axis=0)
        cT = np.ascontiguousarray(cvecs.reshape(1 + NSMP, 8, 128).transpose(2, 1, 0))
        m = dict(shared)
        m.update(xa=np.ascontiguousarray(xa), xs=xs.reshape(NSMP * 128, D), tmask=tm, ck=np.ascontiguousarray(ck[sidx]),
                 cv=np.ascontiguousarray(cv[sidx]), clf=np.ascontiguousarray(clf[sidx]), s0=np.ascontiguousarray(st[sidx]), cT=cT)
        in_maps.append(m)
    res = run_bass_kernel_spmd(nc, in_maps, core_ids=list(range(8)))
    yp = np.zeros((B, SEQ, D), np.float32)
    ys = np.zeros((SB, SS, D), np.float32)
    kp = np.zeros((1, B, SEQ, 8, 64), np.float32)
    vp = np.zeros((1, B, SEQ, 8, 64), np.float32)
    fp = np.zeros((1, B, SEQ, 8), np.float32)
    hp = np.zeros((1, B, 4, 128, 128), np.float32)
    ksm = np.zeros((1, SB, SS, 8, 64), np.float32)
    vsm = np.zeros((1, SB, SS, 8, 64), np.float32)
    fsm = np.zeros((1, SB, SS, 8), np.float32)
    hsm = np.zeros((1, SB, 4, 128, 128), np.float32)
    for c in range(8):
        r = res.results[c]
        b, j = c // 2, c % 2
        for o in range(NOWN):
            p = 2 * (o // 2) + 1
            g0 = p * 256 + (o % 2) * 128 - (256 if j == 0 else 0)
            sl = slice(o * 128, (o + 1) * 128)
            yp[b, g0:g0 + 128] = r["y"][sl]
            kp[0, b, g0:g0 + 128] = r["kn"][sl].reshape(128, 8, 64)
            vp[0, b, g0:g0 + 128] = r["vn"][sl].reshape(128, 8, 64)
            fp[0, b, g0:g0 + 128] = r["lf"][sl]
        if j == 1:
            hp[0, b] = r["hs"][0]
        for s in range(NSMP):
            si = c * NSMP + s
            r0 = (NOWN + s) * 128
            ys[si] = r["y"][r0:r0 + SS]
            ksm[0, si] = r["kn"][r0:r0 + SS].reshape(SS, 8, 64)
            vsm[0, si] = r["vn"][r0:r0 + SS].reshape(SS, 8, 64)
            fsm[0, si] = r["lf"][r0:r0 + SS]
            hsm[0, si] = r["hs"][1 + s]
    return (yp, ys, kp, vp, fp, hp, ksm, vsm, fsm, hsm)
```

```python
import contextlib
import numpy as np
import concourse.bass as bass
import concourse.mybir as mybir
from concourse.bass_utils import run_bass_kernel_spmd

F32 = mybir.dt.float32
BF16 = mybir.dt.bfloat16
AF = mybir.ActivationFunctionType
ALU = mybir.AluOpType
AX = mybir.AxisListType

D = 1024
NE = 256
EPS = 1e-6
C_FQ, C_FK, C_FV, C_FF, C_HQ, C_HF, C_HI, C_HG, C_GA, C_GB = 0, 512, 1024, 1536, 1544, 2056, 2568, 3080, 3592, 4616


class Buf:
    __slots__ = ("name", "w", "r")

    def __init__(self, name=""):
        self.name = name
        self.w = None
        self.r = []


class KB:
    NDMA = 40

    def __init__(self, nc, stack):
        self.nc = nc
        self.stack = stack
        self.E = dict(pe=nc.tensor, act=nc.scalar, dve=nc.vector, pool=nc.gpsimd, sp=nc.sync)
        self.sems = {}
        self.cnt = {}
        for e in self.E:
            self.sems[e] = stack.enter_context(nc.semaphore("prog_" + e))
            self.cnt[e] = 0
        for i in range(self.NDMA):
            k = "dma%d" % i
            self.sems[k] = stack.enter_context(nc.semaphore(k))
            self.cnt[k] = 0
        self.rr = 0
        self.waited = {e: {} for e in self.E}
        self.n_inst = 0

    def sb(self, name, shape, dt):
        return self.stack.enter_context(self.nc.sbuf_tensor(name, list(shape), dt))

    def ps(self, name, shape, dt=F32):
        return self.stack.enter_context(self.nc.psum_tensor(name, list(shape), dt))

    def _wait(self, e, ev):
        if ev is None:
            return
        key, v = ev
        if self.waited[e].get(key, 0) >= v:
            return
        if key == e and e == "pe":
            return
        self.E[e].wait_ge(self.sems[key], v)
        self.waited[e][key] = v

    def _deps(self, e, R, W):
        for b in R:
            self._wait(e, b.w)
        for b in W:
            self._wait(e, b.w)
            for ev in b.r:
                self._wait(e, ev)

    def _commit(self, ev, R, W):
        for b in W:
            b.w = ev
            b.r = []
        for b in R:
            if b.w is not ev:
                b.r.append(ev)
                if len(b.r) > 10:
                    last = {}
                    for kk, v in b.r:
                        if last.get(kk, 0) < v:
                            last[kk] = v
                    b.r = list(last.items())

    def op(self, e, fn, R=(), W=()):
        self._deps(e, R, W)
        ins = fn(self.E[e])
        self.cnt[e] += 1
        ins.then_inc(self.sems[e], 1)
        ev = (e, self.cnt[e])
        self._commit(ev, R, W)
        self.n_inst += 1
        return ev

    def dma(self, q, out, in_, R=(), W=()):
        self._deps(q, R, W)
        k = "dma%d" % self.rr
        self.rr = (self.rr + 1) % self.NDMA
        if self.cnt[k] > 0:
            self._wait(q, (k, self.cnt[k]))
        ins = self.E[q].dma_start(out=out, in_=in_)
        self.cnt[k] += 16
        ins.then_inc(self.sems[k], 16)
        ev = (k, self.cnt[k])
        self._commit(ev, R, W)
        self.n_inst += 1
        return ev

    def barrier(self):
        keys = list(self.sems.keys())
        for e in self.E:
            for kk in keys:
                if kk != e and self.cnt[kk] > 0:
                    self._wait(e, (kk, self.cnt[kk]))

    def finish(self, bufs, e="sp"):
        for b in bufs:
            self._wait(e, b.w)


import os
STOP = int(os.environ.get('KSTOP', '99'))
QZENG = os.environ.get('KQZENG', 'dve')


def build(NPOS, NSMP=2, PAST=1024, moe=True):
    NTA = NPOS * 2
    NOWN = NPOS
    NCT = PAST // 128
    NTK = max(NTA, NCT + 1)
    NMT = NOWN + NSMP
    nc = bass.Bass("TRN2", target_bir_lowering=False)

    def din(name, shape, dt=F32):
        return nc.dram_tensor(name, list(shape), dt, kind="ExternalInput").ap()

    def dout(name, shape, dt=F32):
        return nc.dram_tensor(name, list(shape), dt, kind="ExternalOutput").ap()

    xa = din("xa", [NTA * 128, D])
    xs_in = din("xs", [NSMP * 128, D])
    tmask_in = din("tmask", [128, NTA + NSMP])
    ck = din("ck", [NSMP, PAST, 512])
    cv = din("cv", [NSMP, PAST, 512])
    clf = din("clf", [NSMP, PAST, 8])
    s0 = din("s0", [NSMP, 4, 128, 128])
    cT = din("cT", [128, 8, 1 + NSMP])
    w_ada = din("w_ada", [D, 6 * D])
    b_ada3 = din("b_ada3", [1 + NSMP, 6 * D])
    g13 = din("g13", [1 + NSMP, D])
    g23 = din("g23", [1 + NSMP, D])
    w_in = din("w_in", [D, 5640])
    bffr = din("bffr", [128, 8])
    gqr = din("gqr", [128, 64])
    gkr = din("gkr", [128, 64])
    lbr = din("lbr", [128, 2, 512])
    gor = din("gor", [128, 128])
    w_pa = din("w_pa", [512, D])
    w_pb = din("w_pb", [512, D])
    w_out = din("w_out", [D, D])
    w_rt = din("w_rt", [D, NE])
    brr = din("brr", [128, NE])
    if moe:
        w_eg = din("w_eg", [NE, D, 256])
        w_eu = din("w_eu", [NE, D, 256])
        w_ed = din("w_ed", [NE, 256, D])
    w_sg = din("w_sg", [D, 256])
    w_su = din("w_su", [D, 256])
    w_sd = din("w_sd", [256, D])

    y_out = dout("y", [NMT * 128, D])
    kn_out = dout("kn", [NMT * 128, 512])
    vn_out = dout("vn", [NMT * 128, 512])
    lf_out = dout("lf", [NMT * 128, 8])
    hs_out = dout("hs", [1 + NSMP, 4, 128, 128])
    x1d = nc.dram_tensor("x1d", [NMT * 128, D], F32, kind="Internal").ap()

    with contextlib.ExitStack() as st:
        k = KB(nc, st)
        outbufs = []

        identb = k.sb("identb", [128, 128], BF16)
        identf = k.sb("identf", [128, 128], F32)
        U = k.sb("U", [128, 128], F32)
        ONES = k.sb("ONES", [128, 128], F32)
        LC = k.sb("LC", [128, 128], F32)
        LB1 = k.sb("LB1", [128, 128], F32)
        MT4 = k.sb("MT4", [128, 4, 64], F32)
        TRIB = k.sb("TRIB", [128, 128], BF16)
        SEL3 = k.sb("SEL3", [4, 4, 128], F32)
        SC2 = k.sb("SC2", [128, 2], F32)
        bC = Buf("const")
        k.op("pool", lambda e: e.memset(identf[:], 0.0), W=[bC])
        k.op("pool", lambda e: e.affine_select(out=identf[:], in_=identf[:], pattern=[[-1, 128]], compare_op=ALU.not_equal, fill=1.0, base=0, channel_multiplier=1), R=[bC], W=[bC])
        k.op("pool", lambda e: e.tensor_copy(out=identb[:], in_=identf[:]), R=[bC], W=[bC])
        k.op("pool", lambda e: e.memset(ONES[:], 1.0), W=[bC])
        k.op("pool", lambda e: e.memset(LB1[:], 0.0), W=[bC])
        k.op("pool", lambda e: e.memset(LB1[0:64, 0:64], 1.0), W=[bC])
        k.op("pool", lambda e: e.memset(LB1[64:128, 64:128], 1.0), W=[bC])
        k.op("pool", lambda e: e.affine_select(out=U[:], in_=ONES[:], pattern=[[1, 128]], compare_op=ALU.is_ge, fill=0.0, base=0, channel_multiplier=-1), R=[bC], W=[bC])
        k.op("pool", lambda e: e.tensor_tensor(out=LC[:], in0=U[:], in1=LB1[:], op=ALU.mult), R=[bC], W=[bC])
        k.op("pool", lambda e: e.tensor_copy(out=TRIB[:], in_=U[:]), R=[bC], W=[bC])
        LC4 = k.sb("LC4", [128, 512], F32)
        for h in range(4):
            k.op("pool", lambda e: e.tensor_copy(out=LC4[:, h * 128:(h + 1) * 128], in_=LC[:]), R=[bC], W=[bC])
        for h in range(4):
            k.op("pool", lambda e: e.tensor_copy(out=MT4[0:64, h, :], in_=U[0:64, 0:64]), R=[bC], W=[bC])
            k.op("pool", lambda e: e.tensor_copy(out=MT4[64:128, h, :], in_=U[64:128, 64:128]), R=[bC], W=[bC])
        ZL = k.sb("ZL", [128, 128], BF16)
        ZR = k.sb("ZR", [128, 512], BF16)
        k.op("pool", lambda e: e.memset(ZL[:], 0.0), W=[bC])
        k.op("pool", lambda e: e.memset(ZR[:], 0.0), W=[bC])
        k.op("pool", lambda e: e.memset(SC2[:], 0.0), W=[bC])
        k.op("pool", lambda e: e.memset(SC2[0:64, 0:1], 1.0), W=[bC])
        k.op("pool", lambda e: e.memset(SC2[64:128, 1:2], 1.0), W=[bC])
        k.op("pool", lambda e: e.memset(SEL3[:], 0.0), W=[bC])
        k.op("pool", lambda e: e.affine_select(out=SEL3[:], in_=SEL3[:], pattern=[[-1, 4], [0, 128]], compare_op=ALU.not_equal, fill=1.0, base=0, channel_multiplier=1), R=[bC], W=[bC])

        BFF = k.sb("BFF", [128, 8], F32)
        GQ = k.sb("GQ", [128, 64], F32)
        GK = k.sb("GK", [128, 64], F32)
        LBR = k.sb("LBR", [128, 2, 512], F32)
        OML = k.sb("OML", [128, 512], F32)
        GO = k.sb("GO", [128, 128], F32)
        TM = k.sb("TM", [128, NTA + NSMP], F32)
        KM = k.sb("KM", [128, NTA + NSMP], F32)
        bP = Buf("params")
        k.dma("sp", BFF[:], bffr, W=[bP])
        k.dma("sp", GQ[:], gqr, W=[bP])
        k.dma("sp", GK[:], gkr, W=[bP])
        k.dma("sp", LBR[:], lbr, W=[bP])
        k.dma("sp", GO[:], gor, W=[bP])
        k.dma("sp", TM[:], tmask_in, W=[bP])
        k.op("dve", lambda e: e.tensor_tensor(out=LBR[:, 0, :], in0=LBR[:, 1, :], in1=LBR[:, 0, :], op=ALU.subtract), R=[bP], W=[bP])
        k.op("act", lambda e: e.activation(out=OML[:], in_=LBR[:, 0, :], func=AF.Sigmoid), R=[bP], W=[bP])
        k.op("dve", lambda e: e.tensor_scalar(out=KM[:], in0=TM[:], scalar1=-1.0, scalar2=30000.0, op0=ALU.add, op1=ALU.mult), R=[bP], W=[bP])

        PS = [k.ps("ps%d" % i, [128, 512], F32) for i in range(7)]
        modd = nc.dram_tensor("modd", [1 + NSMP, 6 * D], F32, kind="Internal").ap()
        ktd = nc.dram_tensor("ktd", [NTK, 128, 4, 128], BF16, kind="Internal").ap()
        vad = nc.dram_tensor("vad", [NTK, 128, 8, 128], BF16, kind="Internal").ap()
        bX1 = [Buf() for _ in range(NMT)]
        bPS = [Buf("ps%d" % i) for i in range(7)]
        PB = k.ps("psb", [128, 1024], BF16)
        bPB = Buf("psb")

        NM = 1 + NSMP
        with contextlib.ExitStack() as st0:
            k.stack = st0
            MOD = k.sb("MOD", [NM, 6 * D], F32)
            bMOD = Buf("MOD")
            CT = k.sb("CT", [128, 8, NM], F32)
            bCT = Buf()
            k.dma("sp", CT[:], cT, W=[bCT])
            k.op("act", lambda e: e.activation(out=CT[:], in_=CT[:], func=AF.Silu), R=[bCT], W=[bCT])
            WA = [k.sb("WA%d" % i, [128, 8, 512], F32) for i in range(2)]
            bWA = [Buf(), Buf()]
            B3 = k.sb("B3", [NM, 6 * D], F32)
            G13 = k.sb("G13", [NM, D], F32)
            G23 = k.sb("G23", [NM, D], F32)
            k.dma("sp", B3[:], b_ada3, W=[bMOD])
            k.dma("sp", G13[:], g13, W=[bMOD])
            k.dma("sp", G23[:], g23, W=[bMOD])
            for g in range(12):
                j = g % 2
                k.dma("sp" if j == 0 else "act", WA[j][:], w_ada[:, g * 512:(g + 1) * 512].rearrange("(c p) n -> p c n", p=128), W=[bWA[j]])
                for c in range(8):
                    k.op("pe", lambda e: e.matmul(PS[j][0:NM, :], lhsT=CT[:, c, :], rhs=WA[j][:, c, :], start=(c == 0), stop=(c == 7)), R=[bCT, bWA[j]], W=[bPS[j]])
                k.op("dve", lambda e: e.tensor_tensor(out=MOD[:, g * 512:(g + 1) * 512], in0=PS[j][0:NM, :], in1=B3[:, g * 512:(g + 1) * 512], op=ALU.add), R=[bPS[j], bMOD], W=[bMOD])
            k.op("dve", lambda e: e.scalar_tensor_tensor(out=MOD[:, D:2 * D], in0=MOD[:, D:2 * D], scalar=1.0, in1=G13[:], op0=ALU.add, op1=ALU.mult), R=[bMOD], W=[bMOD])
            k.op("dve", lambda e: e.scalar_tensor_tensor(out=MOD[:, 4 * D:5 * D], in0=MOD[:, 4 * D:5 * D], scalar=1.0, in1=G23[:], op0=ALU.add, op1=ALU.mult), R=[bMOD], W=[bMOD])
            bMODD = Buf("modd")
            k.dma("sp", modd[:, :], MOD[:], R=[bMOD], W=[bMODD])
            k.barrier()
        k.stack = st

        MB = [k.sb("MB%d" % i, [128, D], F32) for i in range(3)]
        bMB = [Buf() for _ in range(3)]

        def load_mod(m, rows):
            for i, row in enumerate(rows):
                k.dma("sp", MB[i][:], modd[m:m + 1, row * D:(row + 1) * D].partition_broadcast(128), R=[bMODD], W=[bMB[i]])

        with contextlib.ExitStack() as st1:
            k.stack = st1
            NFKM = k.sb("NFKM", [128, NTK, 8], F32)
            bNF = Buf("NF")
            BIASALL = k.sb("BIASALL", [128, NTK, 8], F32)
            bBIASALL = Buf()
            CTOT = k.sb("CTOT", [128, 8], F32)
            bCTOT = Buf()
            CREF = k.sb("CREF", [128, 8], F32)
            bCREF = Buf()
            S = k.sb("S", [128, 4, 128], F32)
            bS = Buf("S")
            SBF = k.sb("SBF", [128, 4, 128], BF16)
            bSBF = Buf("SBF")
            XT = [k.sb("XT%d" % i, [128, D], F32) for i in range(2)]
            bXT = [Buf(), Buf()]
            HF32 = k.sb("HF32", [128, D], F32)
            bHF32 = Buf()
            HB = k.sb("HB", [128, D], BF16)
            bHB = Buf()
            HT = [k.sb("HT%d" % i, [128, 8, 128], BF16) for i in range(2)]
            bHT = [Buf(), Buf()]
            WB = [k.sb("WB%d" % i, [128, 8, 512], BF16) for i in range(3)]
            bWB = [Buf(), Buf(), Buf()]
            wrr = [0]
            WFF = k.sb("WFF", [128, 8, 8], BF16)
            bWFF = Buf()
            k.dma("pool", WFF[:], w_in[:, C_FF:C_FF + 8].rearrange("(c p) n -> p c n", p=128), W=[bWFF])
            T = [k.sb("T%d" % i, [128, 512], F32) for i in range(4)]
            bT = [Buf("T%d" % i) for i in range(4)]
            SM = k.sb("SM", [128, 64], F32)
            bSM = Buf("SM")
            EB = [k.sb("EB%d" % i, [128, 512], F32) for i in range(2)]
            bEB = [Buf(), Buf()]
            KH = [k.sb("KH%d" % i, [128, 512], BF16) for i in range(2)]
            bKH = [Buf(), Buf()]
            IB = [k.sb("IB%d" % i, [128, 512], BF16) for i in range(2)]
            bIB = [Buf(), Buf()]
            QH = [k.sb("QH%d" % i, [128, 512], BF16) for i in range(2)]
            bQH = [Buf(), Buf()]
            SG = [k.sb("SG%d" % i, [128, 512], BF16) for i in range(2)]
            bSG = [Buf(), Buf()]
            EBL = [k.sb("EBL%d" % i, [128, 8], F32) for i in range(2)]
            bEBL = [Buf(), Buf()]
            QHT = k.sb("QHT", [128, 4, 128], BF16)
            bQHT = Buf()
            QZ = [k.sb("QZ%d" % i, [128, 4, 128], BF16) for i in range(2)]
            bQZ = [Buf(), Buf()]
            KHZ = [[k.sb("KHZ%d%d" % (i, c), [128, 512], BF16) for c in range(2)] for i in range(2)]
            bKHZ = [[Buf(), Buf()], [Buf(), Buf()]]
            for i in range(2):
                k.op("pool", lambda e: e.memset(QZ[i][:], 0.0), W=[bQZ[i]])
                for c in range(2):
                    k.op("pool", lambda e: e.memset(KHZ[i][c][:], 0.0), W=[bKHZ[i][c]])
            KHT = k.sb("KHT", [128, 4, 128], BF16)
            bKHT = Buf()
            AM = k.sb("AM", [128, 4, 128], BF16)
            bAM = Buf()
            YBT = [k.sb("YBT%d" % i, [128, 4, 128], BF16) for i in range(2)]
            bYBT = [Buf(), Buf()]
            QT = k.sb("QT", [128, 4, 256], BF16)
            bQT = Buf()
            YAT = k.sb("YAT", [64, 8, 256], BF16)
            bYAT = Buf()
            PT = [k.sb("PT%d" % i, [128, 256], BF16) for i in range(3)]
            bPT = [Buf() for _ in range(3)]
            RC = k.sb("RC", [128, 256], F32)
            bRC = Buf()
            UAC = [[k.sb("UAC%d%d" % (i, j), [128, 512], F32) for j in range(2)] for i in range(2)]
            bUAC = [[Buf(), Buf()], [Buf(), Buf()]]
            MG = k.sb("MG", [128, D], BF16)
            bMG = Buf()
            MGT = k.sb("MGT", [128, 8, 128], BF16)
            bMGT = Buf()
            KTS = [k.sb("KTS%d" % i, [128, 4, 128], BF16) for i in range(2)]
            bKTS = [Buf(), Buf()]
            VAS = [k.sb("VAS%d" % i, [128, 8, 128], BF16) for i in range(2)]
            bVAS = [Buf(), Buf()]
            KTW = k.sb("KTW", [128, 4, 128], BF16)
            bKTW = Buf()
            VAW = k.sb("VAW", [128, 8, 128], BF16)
            bVAW = Buf()
            bKTD = [Buf() for _ in range(NTK)]
            bVAD = [Buf() for _ in range(NTK)]
            k.op("pool", lambda e: e.memset(VAW[:], 1.0), W=[bVAW])

            def load_w(src_ap, q="pool", parts=128):
                j = wrr[0] % 3
                wrr[0] += 1
                nh = src_ap.shape[1]
                k.dma(q, WB[j][0:parts, 0:nh, :], src_ap, W=[bWB[j]])
                return WB[j], bWB[j]

            def w_in_cols(c0, n=512):
                return w_in[:, c0:c0 + n].rearrange("(c p) n -> p c n", p=128)

            def proj(ti, W_, bW, bank, ncols=512):
                for c in range(8):
                    k.op("pe", lambda e: e.matmul(PS[bank][:, 0:ncols], lhsT=HT[ti][:, c, :], rhs=W_[:, c, 0:ncols], start=(c == 0), stop=(c == 7)), R=[bHT[ti], bW], W=[bPS[bank]])

            def rms_heads(src, bsrc, nh, hd, G, dst, bdst, scr, bscr):
                s3 = src.rearrange("p (h d) -> p h d", h=nh)
                q3 = scr.rearrange("p (h d) -> p h d", h=nh)
                d3 = dst.rearrange("p (h d) -> p h d", h=nh)
                k.op("pool", lambda e: e.tensor_tensor(out=scr, in0=src, in1=src, op=ALU.mult), R=[bsrc], W=[bscr])
                k.op("dve", lambda e: e.tensor_reduce(out=SM[:, 0:nh], in_=q3, axis=AX.X, op=ALU.add), R=[bscr], W=[bSM])
                k.op("dve", lambda e: e.tensor_scalar(out=SM[:, 0:nh], in0=SM[:, 0:nh], scalar1=1.0 / hd, scalar2=EPS, op0=ALU.mult, op1=ALU.add), R=[bSM], W=[bSM])
                k.op("act", lambda e: e.activation(out=SM[:, 0:nh], in_=SM[:, 0:nh], func=AF.Sqrt), R=[bSM], W=[bSM])
                k.op("dve", lambda e: e.reciprocal(out=SM[:, 0:nh], in_=SM[:, 0:nh]), R=[bSM], W=[bSM])
                k.op("dve", lambda e: e.tensor_tensor(out=q3, in0=s3, in1=SM[:, 0:nh].unsqueeze(2).to_broadcast([128, nh, hd]), op=ALU.mult), R=[bsrc, bSM], W=[bscr])
                k.op("dve", lambda e: e.tensor_tensor(out=d3, in0=q3, in1=G.unsqueeze(1).to_broadcast([128, nh, hd]), op=ALU.mult), R=[bscr, bP], W=[bdst])

            def norm_tile(x_ap, xi, ti, bsrc=None):
                k.dma("sp", XT[xi][:], x_ap, R=([bsrc] if bsrc else []), W=[bXT[xi]])
                k.op("act", lambda e: e.activation(out=HF32[:], in_=XT[xi][:], func=AF.Square, accum_out=SM[:, 32:33]), R=[bXT[xi]], W=[bHF32, bSM])
                k.op("dve", lambda e: e.tensor_scalar(out=SM[:, 32:33], in0=SM[:, 32:33], scalar1=1.0 / D, scalar2=EPS, op0=ALU.mult, op1=ALU.add), R=[bSM], W=[bSM])
                k.op("act", lambda e: e.activation(out=SM[:, 32:33], in_=SM[:, 32:33], func=AF.Sqrt), R=[bSM], W=[bSM])
                k.op("dve", lambda e: e.reciprocal(out=SM[:, 32:33], in_=SM[:, 32:33]), R=[bSM], W=[bSM])
                k.op("dve", lambda e: e.scalar_tensor_tensor(out=HF32[:], in0=XT[xi][:], scalar=SM[:, 32:33], in1=MB[0][:], op0=ALU.mult, op1=ALU.mult), R=[bXT[xi], bSM, bMB[0]], W=[bHF32])
                k.op("pool", lambda e: e.tensor_tensor(out=HB[:], in0=HF32[:], in1=MB[1][:], op=ALU.add), R=[bHF32, bMB[1]], W=[bHB])
                for c in range(8):
                    k.op("pe", lambda e: e.transpose(out=PB[:, c * 128:(c + 1) * 128], in_=HB[:, c * 128:(c + 1) * 128], identity=identb[:]), R=[bHB, bC], W=[bPB])
                k.op("act", lambda e: e.copy(out=HT[ti][:].rearrange("p c t -> p (c t)"), in_=PB[:, :]), R=[bPB], W=[bHT[ti]])

            def store_kt(kt):
                for c in range(4):
                    k.op("pe", lambda e: e.transpose(out=PB[:, c * 128:(c + 1) * 128], in_=HB[:, c * 128:(c + 1) * 128], identity=identb[:]), R=[bHB, bC], W=[bPB])
                k.op("dve", lambda e: e.tensor_copy(out=KTW[:].rearrange("p c t -> p (c t)"), in_=PB[:, 0:512]), R=[bPB], W=[bKTW])
                k.dma("act", ktd[kt], KTW[:], R=[bKTW], W=[bKTD[kt]])

            def cum_logf(kt, km_col, first_ref):
                k.op("pe", lambda e: e.matmul(PS[1][:, 0:8], lhsT=U[:], rhs=SM[:, 8:16], start=True, stop=True), R=[bC, bSM], W=[bPS[1]])
                k.op("pe", lambda e: e.matmul(PS[1][:, 8:16], lhsT=ONES[:], rhs=SM[:, 8:16], start=True, stop=True), R=[bC, bSM], W=[bPS[1]])
                k.op("dve", lambda e: e.scalar_tensor_tensor(out=NFKM[:, kt, :], in0=PS[1][:, 0:8], scalar=km_col, in1=CTOT[:], op0=ALU.add, op1=ALU.add), R=[bPS[1], bP, bCTOT], W=[bNF])
                if first_ref == "mid":
                    k.op("dve", lambda e: e.tensor_tensor(out=CREF[:], in0=PS[1][:, 8:16], in1=CTOT[:], op=ALU.add), R=[bPS[1], bCTOT], W=[bCREF])
                elif first_ref == "start":
                    k.op("dve", lambda e: e.tensor_copy(out=CREF[:], in_=CTOT[:]), R=[bCTOT], W=[bCREF])
                k.op("dve", lambda e: e.tensor_tensor(out=CTOT[:], in0=PS[1][:, 8:16], in1=CTOT[:], op=ALU.add), R=[bPS[1], bCTOT], W=[bCTOT])

            def process_position(x_aps, tcols, kt0, own, orow, qw, smp=None):
                nt = len(x_aps)
                for ti in range(nt):
                    norm_tile(x_aps[ti], ti, ti)
                W_, bW = load_w(w_in_cols(C_HF))
                for ti in range(nt):
                    proj(ti, W_, bW, 0)
                    k.op("act", lambda e: e.activation(out=T[0][:], in_=PS[0][:, :], func=AF.Sigmoid, scale=-1.0), R=[bPS[0]], W=[bT[0]])
                    k.op("dve", lambda e: e.tensor_tensor(out=T[0][:], in0=T[0][:], in1=OML[:], op=ALU.mult), R=[bT[0], bP], W=[bT[0]])
                    k.op("pool", lambda e: e.tensor_scalar(out=T[1][:], in0=T[0][:], scalar1=-1.0, scalar2=1.0, op0=ALU.mult, op1=ALU.add), R=[bT[0]], W=[bT[1]])
                    k.op("act", lambda e: e.activation(out=T[1][:], in_=T[1][:], func=AF.Ln), R=[bT[1]], W=[bT[1]])
                    k.op("pe", lambda e: e.matmul(PS[1][:, :], lhsT=LC[:], rhs=T[1][:], start=True, stop=True), R=[bC, bT[1]], W=[bPS[1]])
                    k.op("act", lambda e: e.copy(out=EB[ti][:], in_=PS[1][:, :]), R=[bPS[1]], W=[bEB[ti]])
                    k.op("act", lambda e: e.activation(out=T[2][:], in_=PS[1][:, :], func=AF.Exp, scale=-1.0), R=[bPS[1]], W=[bT[2]])
                    k.op("dve", lambda e: e.tensor_tensor(out=KH[ti][:], in0=T[0][:], in1=T[2][:], op=ALU.mult), R=[bT[0], bT[2]], W=[bKH[ti]])
                    for c in range(2):
                        k.op("dve", lambda e: e.tensor_tensor(out=KHZ[ti][c][c * 64:(c + 1) * 64, :], in0=T[0][c * 64:(c + 1) * 64, :], in1=T[2][c * 64:(c + 1) * 64, :], op=ALU.mult), R=[bT[0], bT[2]], W=[bKHZ[ti][c]])
                    for h in range(4):
                        k.op("pe", lambda e: e.matmul(PS[2][:, h * 2:h * 2 + 2], lhsT=T[1][:, h * 128:(h + 1) * 128], rhs=SC2[:], start=True, stop=True), R=[bT[1], bC], W=[bPS[2]])
                    k.op("act", lambda e: e.activation(out=EBL[ti][:], in_=PS[2][:, 0:8], func=AF.Exp), R=[bPS[2]], W=[bEBL[ti]])
                if STOP == 1: return
                W_, bW = load_w(w_in_cols(C_HI))
                for ti in range(nt):
                    proj(ti, W_, bW, 0)
                    k.op("act", lambda e: e.activation(out=IB[ti][:], in_=PS[0][:, :], func=AF.Copy, scale=TM[:, tcols[ti]:tcols[ti] + 1]), R=[bPS[0], bP], W=[bIB[ti]])
                if STOP == 2: return
                W_, bW = load_w(w_in_cols(C_FK))
                for ti in range(nt):
                    proj(ti, W_, bW, 0)
                    k.op("act", lambda e: e.copy(out=T[0][:], in_=PS[0][:, :]), R=[bPS[0]], W=[bT[0]])
                    rms_heads(T[0][:], bT[0], 8, 64, GK[:], T[2][:], bT[2], T[1][:], bT[1])
                    if own:
                        outbufs.append(Buf())
                        k.dma("sp", kn_out[(orow + ti) * 128:(orow + ti + 1) * 128, :], T[2][:], R=[bT[2]], W=[outbufs[-1]])
                    k.op("act", lambda e: e.copy(out=HB[:, 0:512], in_=T[2][:]), R=[bT[2]], W=[bHB])
                    store_kt(kt0 + ti)
                if STOP == 21: return
                W_, bW = load_w(w_in_cols(C_FV))
                for ti in range(nt):
                    kt = kt0 + ti
                    proj(ti, W_, bW, 0)
                    k.op("dve", lambda e: e.tensor_copy(out=T[3][:], in_=PS[0][:, :]), R=[bPS[0]], W=[bT[3]])
                    k.op("act", lambda e: e.copy(out=VAW[:, :, 0:64], in_=T[3][:].rearrange("p (h d) -> p h d", h=8)), R=[bT[3]], W=[bVAW])
                    k.dma("act", vad[kt], VAW[:], R=[bVAW], W=[bVAD[kt]])
                    if own:
                        outbufs.append(Buf())
                        k.dma("sp", vn_out[(orow + ti) * 128:(orow + ti + 1) * 128, :], T[3][:], R=[bT[3]], W=[outbufs[-1]])
                if STOP == 3: return
                for ti in range(nt):
                    proj(ti, WFF, bWFF, 0, ncols=8)
                    k.op("dve", lambda e: e.tensor_tensor(out=SM[:, 0:8], in0=PS[0][:, 0:8], in1=BFF[:], op=ALU.add), R=[bPS[0], bP], W=[bSM])
                    k.op("act", lambda e: e.activation(out=SM[:, 0:8], in_=SM[:, 0:8], func=AF.Exp, scale=-1.0), R=[bSM], W=[bSM])
                    k.op("act", lambda e: e.activation(out=SM[:, 8:16], in_=SM[:, 0:8], func=AF.Ln, bias=1.0), R=[bSM], W=[bSM])
                    if own:
                        k.op("dve", lambda e: e.tensor_scalar(out=SM[:, 16:24], in0=SM[:, 8:16], scalar1=-1.0, scalar2=None, op0=ALU.mult), R=[bSM], W=[bSM])
                        outbufs.append(Buf())
                        k.dma("sp", lf_out[(orow + ti) * 128:(orow + ti + 1) * 128, :], SM[:, 16:24], R=[bSM], W=[outbufs[-1]])
                    cum_logf(kt0 + ti, KM[:, tcols[ti]:tcols[ti] + 1], ("mid" if nt == 2 else "start") if ti == 0 else None)
                if own:
                    W_, bW = load_w(w_in_cols(C_HQ))
                    for ti in range(nt):
                        proj(ti, W_, bW, 0)
                        k.op("act", lambda e: e.activation(out=T[0][:], in_=PS[0][:, :], func=AF.Silu), R=[bPS[0]], W=[bT[0]])
                        k.op("act", lambda e: e.activation(out=T[1][:], in_=EB[ti][:], func=AF.Exp), R=[bEB[ti]], W=[bT[1]])
                        k.op("dve", lambda e: e.tensor_tensor(out=QH[ti][:], in0=T[0][:], in1=T[1][:], op=ALU.mult), R=[bT[0], bT[1]], W=[bQH[ti]])
                    W_, bW = load_w(w_in_cols(C_HG))
                    for ti in range(nt):
                        proj(ti, W_, bW, 0)
                        k.op("act", lambda e: e.activation(out=SG[ti][:], in_=PS[0][:, :], func=AF.Sigmoid), R=[bPS[0]], W=[bSG[ti]])
                if STOP == 4: return
                for ti in range(nt):
                    if STOP in (51, 52) and not own: return
                    if own and STOP != 55:
                        for h in range(4):
                            k.op("pe", lambda e: e.transpose(out=PB[:, h * 128:(h + 1) * 128], in_=QH[ti][:, h * 128:(h + 1) * 128], identity=identb[:]), R=[bQH[ti], bC], W=[bPB])
                            k.op("pe", lambda e: e.transpose(out=PB[:, 512 + h * 128:512 + (h + 1) * 128], in_=KH[ti][:, h * 128:(h + 1) * 128], identity=identb[:]), R=[bKH[ti], bC], W=[bPB])
                        k.op("act", lambda e: e.copy(out=QHT[:].rearrange("p h t -> p (h t)"), in_=PB[:, 0:512]), R=[bPB], W=[bQHT])
                        if STOP == 511: return
                        for c in range(2):
                            k.op(QZENG, lambda e: e.tensor_copy(out=QZ[c][:, :, c * 64:(c + 1) * 64], in_=QHT[:, :, c * 64:(c + 1) * 64]), R=[bQHT], W=[bQZ[c]])
                        if STOP == 512: return
                        k.op("dve", lambda e: e.tensor_copy(out=KHT[:].rearrange("p h t -> p (h t)"), in_=PB[:, 512:1024]), R=[bPB], W=[bKHT])
                        if STOP == 51: return
                        for h in range(4):
                            k.op("pe", lambda e: e.matmul(PS[3][:, h * 128:(h + 1) * 128], lhsT=KHT[:, h, :], rhs=QHT[:, h, :], start=True, stop=True), R=[bKHT, bQHT], W=[bPS[3]])
                        k.op("dve", lambda e: e.tensor_tensor(out=AM[:].rearrange("p h t -> p (h t)"), in0=PS[3][:, :], in1=LC4[:], op=ALU.mult), R=[bPS[3], bC], W=[bAM])
                        if STOP == 52: return
                        k.op("pe", lambda e: e.matmul(PS[4][:, :], lhsT=ZL[:], rhs=ZR[:], start=True, stop=False), R=[bC], W=[bPS[4]])
                        for h in range(4):
                            k.op("pe", lambda e: e.matmul(PS[4][:, h * 128:(h + 1) * 128], lhsT=AM[:, h, :], rhs=IB[ti][:, h * 128:(h + 1) * 128], start=False, stop=False, skip_group_check=True), R=[bAM, bIB[ti]], W=[bPS[4]])
                            k.op("pe", lambda e: e.matmul(PS[4][:, h * 128:(h + 1) * 128], lhsT=QZ[0][:, h, :], rhs=SBF[:, h, :], start=False, stop=(smp is not None and h == 3), skip_group_check=True), R=[bQZ[0], bSBF], W=[bPS[4]])
                    if STOP == 53: return
                    nch = 1 if smp is not None else 2
                    for c in range(nch):
                        if own and c == 1 and STOP != 55:
                            for h in range(4):
                                k.op("pe", lambda e: e.matmul(PS[4][:, h * 128:(h + 1) * 128], lhsT=QZ[1][:, h, :], rhs=SBF[:, h, :], start=False, stop=(h == 3), skip_group_check=True), R=[bQZ[1], bSBF], W=[bPS[4]])
                        for h in range(4):
                            k.op("pe", lambda e: e.matmul(PS[5][:, h * 128:(h + 1) * 128], lhsT=KHZ[ti][c][:, h * 128:(h + 1) * 128], rhs=IB[ti][:, h * 128:(h + 1) * 128], start=True, stop=True), R=[bKHZ[ti][c], bIB[ti]], W=[bPS[5]])
                        k.op("dve", lambda e: e.tensor_tensor(out=S[:].rearrange("p h e -> p (h e)"), in0=PS[5][:, :], in1=S[:].rearrange("p h e -> p (h e)"), op=ALU.add), R=[bPS[5], bS], W=[bS])
                        k.op("dve", lambda e: e.tensor_tensor(out=S[:], in0=S[:], in1=EBL[ti][:].rearrange("p (h c) -> p h c", c=2)[:, :, c:c + 1].to_broadcast([128, 4, 128]), op=ALU.mult), R=[bS, bEBL[ti]], W=[bS])
                        k.op("act", lambda e: e.copy(out=SBF[:], in_=S[:]), R=[bS], W=[bSBF])
                    if STOP == 54: return
                    if STOP == 55: continue
                    if own:
                        k.op("act", lambda e: e.copy(out=T[0][:], in_=PS[4][:, :]), R=[bPS[4]], W=[bT[0]])
                        rms_heads(T[0][:], bT[0], 4, 128, GO[:], T[2][:], bT[2], T[1][:], bT[1])
                        k.op("dve", lambda e: e.tensor_tensor(out=HB[:, 0:512], in0=T[2][:], in1=SG[ti][:], op=ALU.mult), R=[bT[2], bSG[ti]], W=[bHB])
                        for h in range(4):
                            k.op("pe", lambda e: e.transpose(out=PB[:, h * 128:(h + 1) * 128], in_=HB[:, h * 128:(h + 1) * 128], identity=identb[:]), R=[bHB, bC], W=[bPB])
                        k.op("act", lambda e: e.copy(out=YBT[ti][:].rearrange("p h t -> p (h t)"), in_=PB[:, 0:512]), R=[bPB], W=[bYBT[ti]])
                if smp is not None:
                    outbufs.append(Buf())
                    k.dma("sp", hs_out[1 + smp].rearrange("h d e -> d h e"), S[:], R=[bS], W=[outbufs[-1]])
                if not own:
                    return
                if STOP == 5: return
                W_, bW = load_w(w_in_cols(C_FQ))
                for ti in range(nt):
                    proj(ti, W_, bW, 0)
                    k.op("act", lambda e: e.copy(out=T[0][:], in_=PS[0][:, :]), R=[bPS[0]], W=[bT[0]])
                    rms_heads(T[0][:], bT[0], 8, 64, GQ[:], T[2][:], bT[2], T[1][:], bT[1])
                    k.op("act", lambda e: e.copy(out=HB[:, 0:512], in_=T[2][:]), R=[bT[2]], W=[bHB])
                    for c in range(4):
                        k.op("pe", lambda e: e.transpose(out=PB[:, c * 128:(c + 1) * 128], in_=HB[:, c * 128:(c + 1) * 128], identity=identb[:]), R=[bHB, bC], W=[bPB])
                    k.op("dve", lambda e: e.tensor_copy(out=QT[:, :, ti * 128:(ti + 1) * 128], in_=PB[:, 0:512].rearrange("p (c t) -> p c t", c=4)), R=[bPB], W=[bQT])
                if STOP == 6: return
                nkt = kt0 + nt
                k.op("dve", lambda e: e.tensor_tensor(out=BIASALL[:, 0:nkt, :], in0=NFKM[:, 0:nkt, :], in1=CREF[:].unsqueeze(1).to_broadcast([128, nkt, 8]), op=ALU.subtract), R=[bNF, bCREF], W=[bBIASALL])
                pi = 0
                for ob in range(4):
                    k.op("pe", lambda e: e.matmul(PS[ob][:, :], lhsT=ZL[:], rhs=ZR[:], start=True, stop=False), R=[bC], W=[bPS[ob]])
                for kt in range(nkt):
                    j = kt % 2
                    k.dma("sp", KTS[j][:], ktd[kt], R=[bKTD[kt]], W=[bKTS[j]])
                    k.dma("sp", VAS[j][:], vad[kt], R=[bVAD[kt]], W=[bVAS[j]])
                    dj = kt - kt0
                    q0 = 0 if dj < 0 else dj * 128
                    nq = qw - q0
                    for h in range(8):
                        hp, ho = h // 2, (h % 2) * 64
                        ob, oc = h // 2, (h % 2) * 256
                        sb_ = 4 + (pi % 2)
                        p_, bp_ = PT[pi % 3], bPT[pi % 3]
                        pi += 1
                        k.op("pe", lambda e: e.matmul(PS[sb_][:, 0:nq], lhsT=KTS[j][ho:ho + 64, hp, :], rhs=QT[ho:ho + 64, hp, q0:qw], start=True, stop=True), R=[bKTS[j], bQT], W=[bPS[sb_]])
                        k.op("act", lambda e: e.activation(out=p_[:, 0:nq], in_=PS[sb_][:, 0:nq], func=AF.Exp, bias=BIASALL[:, kt, h:h + 1], scale=0.125), R=[bPS[sb_], bBIASALL], W=[bp_])
                        if dj >= 0:
                            k.op("dve", lambda e: e.tensor_tensor(out=p_[:, 0:128], in0=p_[:, 0:128], in1=TRIB[:], op=ALU.mult), R=[bp_, bC], W=[bp_])
                        k.op("pe", lambda e: e.matmul(PS[ob][:, oc + q0:oc + qw], lhsT=VAS[j][:, h, :], rhs=p_[:, 0:nq], start=False, stop=(kt == nkt - 1 and h % 2 == 1), skip_group_check=True), R=[bVAS[j], bp_], W=[bPS[ob]])
                for h in range(8):
                    ob, oc = h // 2, (h % 2) * 256
                    k.op("dve", lambda e: e.reciprocal(out=RC[64:128, 0:qw], in_=PS[ob][64:128, oc:oc + qw]), R=[bPS[ob]], W=[bRC])
                    k.op("dve", lambda e: e.tensor_tensor(out=YAT[:, h, 0:qw], in0=PS[ob][0:64, oc:oc + qw], in1=RC[64:128, 0:qw], op=ALU.mult), R=[bPS[ob], bRC], W=[bYAT])
                if STOP == 7: return
                for half in range(2):
                    Wg_, bWg = load_w(w_in_cols(C_GA + half * 512))
                    Wp_, bWp = load_w(w_pa[:, half * 512:(half + 1) * 512].rearrange("(h p) n -> p h n", p=64), parts=64)
                    for ti in range(nt):
                        proj(ti, Wg_, bWg, 0)
                        k.op("act", lambda e: e.activation(out=T[0][:], in_=PS[0][:, :], func=AF.Sigmoid), R=[bPS[0]], W=[bT[0]])
                        for h in range(8):
                            k.op("pe", lambda e: e.matmul(PS[1][:, :], lhsT=YAT[:, h, ti * 128:(ti + 1) * 128], rhs=Wp_[0:64, h, :], start=(h == 0), stop=(h == 7)), R=[bYAT, bWp], W=[bPS[1]])
                        k.op("dve", lambda e: e.tensor_tensor(out=UAC[ti][half][:], in0=PS[1][:, :], in1=T[0][:], op=ALU.mult), R=[bPS[1], bT[0]], W=[bUAC[ti][half]])
                for half in range(2):
                    Wg_, bWg = load_w(w_in_cols(C_GB + half * 512))
                    Wp_, bWp = load_w(w_pb[:, half * 512:(half + 1) * 512].rearrange("(h p) n -> p h n", p=128))
                    for ti in range(nt):
                        proj(ti, Wg_, bWg, 0)
                        k.op("act", lambda e: e.activation(out=T[0][:], in_=PS[0][:, :], func=AF.Sigmoid), R=[bPS[0]], W=[bT[0]])
                        for h in range(4):
                            k.op("pe", lambda e: e.matmul(PS[1][:, :], lhsT=YBT[ti][:, h, :], rhs=Wp_[:, h, :], start=(h == 0), stop=(h == 3)), R=[bYBT[ti], bWp], W=[bPS[1]])
                        k.op("dve", lambda e: e.tensor_tensor(out=T[1][:], in0=PS[1][:, :], in1=T[0][:], op=ALU.mult), R=[bPS[1], bT[0]], W=[bT[1]])
                        k.op("pool", lambda e: e.tensor_tensor(out=UAC[ti][half][:], in0=UAC[ti][half][:], in1=T[1][:], op=ALU.add), R=[bT[1], bUAC[ti][half]], W=[bUAC[ti][half]])
                Wo = [load_w(w_out[:, half * 512:(half + 1) * 512].rearrange("(c p) n -> p c n", p=128)) for half in range(2)]
                for ti in range(nt):
                    for half in range(2):
                        k.op("act", lambda e: e.copy(out=MG[:, half * 512:(half + 1) * 512], in_=UAC[ti][half][:]), R=[bUAC[ti][half]], W=[bMG])
                    for c in range(8):
                        k.op("pe", lambda e: e.transpose(out=PB[:, c * 128:(c + 1) * 128], in_=MG[:, c * 128:(c + 1) * 128], identity=identb[:]), R=[bMG, bC], W=[bPB])
                    k.op("act", lambda e: e.copy(out=MGT[:].rearrange("p c t -> p (c t)"), in_=PB[:, :]), R=[bPB], W=[bMGT])
                    k.dma("sp", XT[ti][:], x_aps[ti], W=[bXT[ti]])
                    for half in range(2):
                        Wo_, bWo = Wo[half]
                        for c in range(8):
                            k.op("pe", lambda e: e.matmul(PS[1][:, :], lhsT=MGT[:, c, :], rhs=Wo_[:, c, :], start=(c == 0), stop=(c == 7)), R=[bMGT, bWo], W=[bPS[1]])
                        k.op("dve", lambda e: e.tensor_tensor(out=T[0][:], in0=PS[1][:, :], in1=MB[2][:, half * 512:(half + 1) * 512], op=ALU.mult), R=[bPS[1], bMB[2]], W=[bT[0]])
                        k.op("pool", lambda e: e.tensor_tensor(out=XT[ti][:, half * 512:(half + 1) * 512], in0=XT[ti][:, half * 512:(half + 1) * 512], in1=T[0][:], op=ALU.add), R=[bT[0], bXT[ti]], W=[bXT[ti]])
                    k.dma("sp", x1d[(orow + ti) * 128:(orow + ti + 1) * 128, :], XT[ti][:], R=[bXT[ti]], W=[bX1[orow + ti]])

            k.op("pool", lambda e: e.memset(S[:], 0.0), W=[bS])
            k.op("pool", lambda e: e.memset(SBF[:], 0.0), W=[bSBF])
            k.op("pool", lambda e: e.memset(CTOT[:], 0.0), W=[bCTOT])
            load_mod(0, [1, 0, 2])
            for p in range(NPOS):
                own = (p % 2 == 1)
                process_position([xa[(2 * p + i) * 128:(2 * p + i + 1) * 128, :] for i in range(2)], [2 * p, 2 * p + 1], 2 * p, own, (p // 2) * 2, 256)
            outbufs.append(Buf())
            k.dma("sp", hs_out[0].rearrange("h d e -> d h e"), S[:], R=[bS], W=[outbufs[-1]])
            for s in range(NSMP):
                load_mod(1 + s, [1, 0, 2])
                k.op("pool", lambda e: e.memset(CTOT[:], 0.0), W=[bCTOT])
                k.dma("sp", S[:], s0[s].rearrange("h d e -> d h e"), W=[bS])
                k.op("act", lambda e: e.copy(out=SBF[:], in_=S[:]), R=[bS], W=[bSBF])
                for ct in range(NCT):
                    k.dma("sp", T[0][:], ck[s, ct * 128:(ct + 1) * 128, :], W=[bT[0]])
                    k.op("act", lambda e: e.copy(out=HB[:, 0:512], in_=T[0][:]), R=[bT[0]], W=[bHB])
                    store_kt(ct)
                    k.dma("sp", T[3][:], cv[s, ct * 128:(ct + 1) * 128, :], W=[bT[3]])
                    k.op("act", lambda e: e.copy(out=VAW[:, :, 0:64], in_=T[3][:].rearrange("p (h d) -> p h d", h=8)), R=[bT[3]], W=[bVAW])
                    k.dma("act", vad[ct], VAW[:], R=[bVAW], W=[bVAD[ct]])
                    k.dma("sp", SM[:, 0:8], clf[s, ct * 128:(ct + 1) * 128, :], W=[bSM])
                    k.op("dve", lambda e: e.tensor_scalar(out=SM[:, 8:16], in0=SM[:, 0:8], scalar1=-1.0, scalar2=None, op0=ALU.mult), R=[bSM], W=[bSM])
                    cum_logf(ct, 0.0, None)
                process_position([xs_in[s * 128:(s + 1) * 128, :]], [NTA + s], NCT, True, NOWN + s, 128, smp=s)
            k.barrier()
        k.stack = st

        if moe:
            GT = 12
            with contextlib.ExitStack() as st2:
                k.stack = st2
                WR = k.sb("WR", [128, 8, NE], BF16)
                bWR = Buf()
                k.dma("pool", WR[:], w_rt.rearrange("(c p) n -> p c n", p=128), W=[bWR])
                BR = k.sb("BR", [128, NE], F32)
                k.dma("sp", BR[:], brr, W=[bP])
                H2T = k.sb("H2T", [128, 8, GT * 128], BF16)
                bH2T = Buf()
                YACC = [k.sb("YACC%d" % i, [128, D], F32) for i in range(GT)]
                bYACC = [Buf() for _ in range(GT)]
                GG = [k.sb("GG%d" % i, [128, NE + 1], F32) for i in range(GT)]
                bGG = [Buf() for _ in range(GT)]
                XT2 = k.sb("XT2", [128, D], F32)
                bXT2 = Buf()
                HF2 = k.sb("HF2", [128, D], F32)
                bHF2 = Buf()
                HB2 = k.sb("HB2", [128, D], BF16)
                bHB2 = Buf()
                R1 = k.sb("R1", [128, NE], F32)
                bR1 = Buf()
                R2 = k.sb("R2", [128, NE], F32)
                bR2 = Buf()
                R3 = k.sb("R3", [128, NE], F32)
                bR3 = Buf()
                SM2 = k.sb("SM2", [128, 64], F32)
                bSM2 = Buf()
                WG = [k.sb("WG%d" % i, [128, 8, 256], BF16) for i in range(2)]
                WU = [k.sb("WU%d" % i, [128, 8, 256], BF16) for i in range(2)]
                WD = [k.sb("WD%d" % i, [128, 2, D], BF16) for i in range(2)]
                bWE = [Buf(), Buf()]
                AS = k.sb("AS", [128, 2, 512], BF16)
                bAS = Buf()
                AA = [k.sb("AA%d" % i, [128, 2, 512], BF16) for i in range(2)]
                bAA = [Buf(), Buf()]
                ngroups = (NMT + GT - 1) // GT
                cur_m = [-1]
                for g in range(ngroups):
                    tiles = list(range(g * GT, min(NMT, (g + 1) * GT)))
                    ntg = len(tiles)
                    for li, t in enumerate(tiles):
                        m = 0 if t < NOWN else 1 + (t - NOWN)
                        if m != cur_m[0]:
                            load_mod(m, [4, 3, 5])
                            cur_m[0] = m
                        k.dma("sp", XT2[:], x1d[t * 128:(t + 1) * 128, :], R=[bX1[t]], W=[bXT2])
                        k.op("act", lambda e: e.activation(out=HF2[:], in_=XT2[:], func=AF.Square, accum_out=SM2[:, 32:33]), R=[bXT2], W=[bHF2, bSM2])
                        k.op("dve", lambda e: e.tensor_scalar(out=SM2[:, 32:33], in0=SM2[:, 32:33], scalar1=1.0 / D, scalar2=EPS, op0=ALU.mult, op1=ALU.add), R=[bSM2], W=[bSM2])
                        k.op("act", lambda e: e.activation(out=SM2[:, 32:33], in_=SM2[:, 32:33], func=AF.Sqrt), R=[bSM2], W=[bSM2])
                        k.op("dve", lambda e: e.reciprocal(out=SM2[:, 32:33], in_=SM2[:, 32:33]), R=[bSM2], W=[bSM2])
                        k.op("dve", lambda e: e.scalar_tensor_tensor(out=HF2[:], in0=XT2[:], scalar=SM2[:, 32:33], in1=MB[0][:], op0=ALU.mult, op1=ALU.mult), R=[bXT2, bSM2, bMB[0]], W=[bHF2])
                        k.op("pool", lambda e: e.tensor_tensor(out=HB2[:], in0=HF2[:], in1=MB[1][:], op=ALU.add), R=[bHF2, bMB[1]], W=[bHB2])
                        for c in range(8):
                            k.op("pe", lambda e: e.transpose(out=PB[:, c * 128:(c + 1) * 128], in_=HB2[:, c * 128:(c + 1) * 128], identity=identb[:]), R=[bHB2, bC], W=[bPB])
                        k.op("act", lambda e: e.copy(out=H2T[:, :, li * 128:(li + 1) * 128], in_=PB[:, :].rearrange("p (c t) -> p c t", c=8)), R=[bPB], W=[bH2T])
                        k.op("pool", lambda e: e.memset(YACC[li][:], 0.0), W=[bYACC[li]])
                        for c in range(8):
                            k.op("pe", lambda e: e.matmul(PS[6][:, 0:NE], lhsT=H2T[:, c, li * 128:(li + 1) * 128], rhs=WR[:, c, :], start=(c == 0), stop=(c == 7)), R=[bH2T, bWR], W=[bPS[6]])
                        k.op("act", lambda e: e.activation(out=R1[:], in_=PS[6][:, 0:NE], func=AF.Sigmoid), R=[bPS[6]], W=[bR1])
                        k.op("dve", lambda e: e.tensor_tensor(out=R2[:], in0=R1[:], in1=BR[:], op=ALU.add), R=[bR1, bP], W=[bR2])
                        r2g = R2[:].rearrange("p (g e) -> p g e", g=8)
                        r3g = R3[:].rearrange("p (g e) -> p g e", g=8)
                        k.op("dve", lambda e: e.tensor_reduce(out=SM2[:, 0:8], in_=r2g, axis=AX.X, op=ALU.max), R=[bR2], W=[bSM2])
                        k.op("dve", lambda e: e.tensor_tensor(out=r3g, in0=r2g, in1=SM2[:, 0:8].unsqueeze(2).to_broadcast([128, 8, 32]), op=ALU.is_equal), R=[bR2, bSM2], W=[bR3])
                        k.op("dve", lambda e: e.scalar_tensor_tensor(out=R3[:], in0=R3[:], scalar=-1.0e4, in1=R2[:], op0=ALU.mult, op1=ALU.add), R=[bR3, bR2], W=[bR3])
                        k.op("dve", lambda e: e.tensor_reduce(out=SM2[:, 8:16], in_=r3g, axis=AX.X, op=ALU.max), R=[bR3], W=[bSM2])
                        k.op("dve", lambda e: e.tensor_tensor(out=SM2[:, 0:8], in0=SM2[:, 0:8], in1=SM2[:, 8:16], op=ALU.add), R=[bSM2], W=[bSM2])
                        k.op("dve", lambda e: e.max(out=SM2[:, 16:24], in_=SM2[:, 0:8]), R=[bSM2], W=[bSM2])
                        k.op("dve", lambda e: e.tensor_scalar(out=SM2[:, 0:8], in0=SM2[:, 0:8], scalar1=SM2[:, 19:20], scalar2=None, op0=ALU.is_ge), R=[bSM2], W=[bSM2])
                        k.op("dve", lambda e: e.tensor_scalar(out=SM2[:, 0:8], in0=SM2[:, 0:8], scalar1=-1.0, scalar2=1.0e4, op0=ALU.add, op1=ALU.mult), R=[bSM2], W=[bSM2])
                        k.op("dve", lambda e: e.tensor_tensor(out=r3g, in0=r2g, in1=SM2[:, 0:8].unsqueeze(2).to_broadcast([128, 8, 32]), op=ALU.add), R=[bR2, bSM2], W=[bR3])
                        k.op("dve", lambda e: e.max(out=SM2[:, 24:32], in_=R3[:]), R=[bR3], W=[bSM2])
                        k.op("dve", lambda e: e.tensor_scalar(out=R3[:], in0=R3[:], scalar1=SM2[:, 31:32], scalar2=None, op0=ALU.is_ge), R=[bR3, bSM2], W=[bR3])
                        k.op("dve", lambda e: e.tensor_tensor(out=R3[:], in0=R3[:], in1=R1[:], op=ALU.mult), R=[bR3, bR1], W=[bR3])
                        k.op("dve", lambda e: e.tensor_reduce(out=SM2[:, 40:41], in_=R3[:], axis=AX.X, op=ALU.add), R=[bR3], W=[bSM2])
                        k.op("dve", lambda e: e.reciprocal(out=SM2[:, 40:41], in_=SM2[:, 40:41]), R=[bSM2], W=[bSM2])
                        k.op("dve", lambda e: e.tensor_scalar(out=GG[li][:, 0:NE], in0=R3[:], scalar1=SM2[:, 40:41], scalar2=2.5, op0=ALU.mult, op1=ALU.mult), R=[bR3, bSM2], W=[bGG[li]])
                        k.op("pool", lambda e: e.memset(GG[li][:, NE:NE + 1], 1.0), W=[bGG[li]])
                    ntok = ntg * 128
                    chunks = [(c0, min(512, ntok - c0)) for c0 in range(0, ntok, 512)]
                    for ex in range(NE + 1):
                        j = ex % 2
                        if ex < NE:
                            sg_, su_, sd_ = w_eg[ex], w_eu[ex], w_ed[ex]
                        else:
                            sg_, su_, sd_ = w_sg, w_su, w_sd
                        k.dma("pool", WG[j][:], sg_.rearrange("(c p) n -> p c n", p=128), W=[bWE[j]])
                        k.dma("pool", WU[j][:], su_.rearrange("(c p) n -> p c n", p=128), W=[bWE[j]])
                        k.dma("pool", WD[j][:], sd_.rearrange("(c p) n -> p c n", p=128), W=[bWE[j]])
                        for ci, (c0, cn) in enumerate(chunks):
                            for fc in range(2):
                                for c in range(8):
                                    k.op("pe", lambda e: e.matmul(PS[fc][:, 0:cn], lhsT=WG[j][:, c, fc * 128:(fc + 1) * 128], rhs=H2T[:, c, c0:c0 + cn], start=(c == 0), stop=(c == 7)), R=[bWE[j], bH2T], W=[bPS[fc]])
                                for c in range(8):
                                    k.op("pe", lambda e: e.matmul(PS[2 + fc][:, 0:cn], lhsT=WU[j][:, c, fc * 128:(fc + 1) * 128], rhs=H2T[:, c, c0:c0 + cn], start=(c == 0), stop=(c == 7)), R=[bWE[j], bH2T], W=[bPS[2 + fc]])
                            aj = ci % 2
                            for fc in range(2):
                                k.op("act", lambda e: e.activation(out=AS[:, fc, 0:cn], in_=PS[fc][:, 0:cn], func=AF.Silu), R=[bPS[fc]], W=[bAS])
                                k.op("dve", lambda e: e.tensor_tensor(out=AA[aj][:, fc, 0:cn], in0=PS[2 + fc][:, 0:cn], in1=AS[:, fc, 0:cn], op=ALU.mult), R=[bPS[2 + fc], bAS], W=[bAA[aj]])
                            for tt in range(cn // 128):
                                li = (c0 // 128) + tt
                                for hf in range(2):
                                    for fc in range(2):
                                        k.op("pe", lambda e: e.matmul(PS[4 + hf][:, :], lhsT=AA[aj][:, fc, tt * 128:(tt + 1) * 128], rhs=WD[j][:, fc, hf * 512:(hf + 1) * 512], start=(fc == 0), stop=(fc == 1)), R=[bAA[aj], bWE[j]], W=[bPS[4 + hf]])
                                    k.op("dve", lambda e: e.scalar_tensor_tensor(out=YACC[li][:, hf * 512:(hf + 1) * 512], in0=PS[4 + hf][:, :], scalar=GG[li][:, ex:ex + 1], in1=YACC[li][:, hf * 512:(hf + 1) * 512], op0=ALU.mult, op1=ALU.add), R=[bPS[4 + hf], bGG[li], bYACC[li]], W=[bYACC[li]])
                    for li, t in enumerate(tiles):
                        m = 0 if t < NOWN else 1 + (t - NOWN)
                        if m != cur_m[0]:
                            load_mod(m, [4, 3, 5])
                            cur_m[0] = m
                        k.dma("sp", XT2[:], x1d[t * 128:(t + 1) * 128, :], R=[bX1[t]], W=[bXT2])
                        k.op("dve", lambda e: e.tensor_tensor(out=YACC[li][:], in0=YACC[li][:], in1=MB[2][:], op=ALU.mult), R=[bYACC[li], bMB[2]], W=[bYACC[li]])
                        k.op("pool", lambda e: e.tensor_tensor(out=YACC[li][:], in0=YACC[li][:], in1=XT2[:], op=ALU.add), R=[bYACC[li], bXT2], W=[bYACC[li]])
                        outbufs.append(Buf())
                        k.dma("sp", y_out[t * 128:(t + 1) * 128, :], YACC[li][:], R=[bYACC[li]], W=[outbufs[-1]])
                k.barrier()
            k.stack = st
        if not moe:
            for t in range(NMT):
                outbufs.append(Buf())
                k.dma("sp", y_out[t * 128:(t + 1) * 128, :], x1d[t * 128:(t + 1) * 128, :], R=[bX1[t]], W=[outbufs[-1]])
        k.finish(outbufs, "sp")
        k.barrier()
    return nc


_NC_CACHE = {}


def _rep(a, n=128):
    return np.ascontiguousarray(np.broadcast_to(np.asarray(a, np.float32)[None], (n,) + tuple(a.shape)))


def kernel(x_prompt, x_sample, cache_fox_k, cache_fox_v, cache_fox_logf, state_hgrn, c_prompt, c_sample,
           w_ada, b_ada, g_norm1, w_in, b_fox_f, g_q, g_k, hgrn_lb, g_hgrn_o, w_proj_a, w_proj_b, w_out,
           g_norm2, w_router, b_router, w_exp_gate, w_exp_up, w_exp_down, w_sh_gate, w_sh_up, w_sh_down, _moe=True):
    f = lambda a: np.ascontiguousarray(np.asarray(a, dtype=np.float32))
    x_prompt, x_sample = f(x_prompt), f(x_sample)
    B, SEQ, _ = x_prompt.shape
    SB, SS, _ = x_sample.shape
    PAST = cache_fox_k.shape[2]
    NPOS = SEQ // 256
    NSMP = SB // 8
    NTA, NOWN, NMT = NPOS * 2, NPOS, NPOS + NSMP
    key = (NPOS, NSMP, PAST, _moe)
    if key not in _NC_CACHE:
        _NC_CACHE[key] = build(NPOS, NSMP, PAST, moe=_moe)
    nc = _NC_CACHE[key]
    ck = f(cache_fox_k)[0].reshape(SB, PAST, 512)
    cv = f(cache_fox_v)[0].reshape(SB, PAST, 512)
    clf = f(cache_fox_logf)[0]
    st = f(state_hgrn)[0]
    shared = dict(
        w_ada=f(w_ada)[0], w_in=f(w_in)[0], bffr=_rep(f(b_fox_f)[0]), gqr=_rep(f(g_q)[0]), gkr=_rep(f(g_k)[0]),
        lbr=_rep(f(hgrn_lb)), gor=_rep(f(g_hgrn_o)[0]), w_pa=f(w_proj_a)[0], w_pb=f(w_proj_b)[0], w_out=f(w_out)[0],
        w_rt=f(w_router)[0], brr=_rep(f(b_router)[0]), w_eg=f(w_exp_gate)[0], w_eu=f(w_exp_up)[0], w_ed=f(w_exp_down)[0],
        w_sg=f(w_sh_gate)[0], w_su=f(w_sh_up)[0], w_sd=f(w_sh_down)[0],
        b_ada3=_rep(f(b_ada)[0], 1 + NSMP), g13=_rep(f(g_norm1)[0], 1 + NSMP), g23=_rep(f(g_norm2)[0], 1 + NSMP))
    if not _moe:
        for nm in ("w_eg", "w_eu", "w_ed"):
            shared.pop(nm)
    in_maps = []
    for c in range(8):
        b, j = c // 2, c % 2
        if j == 1:
            xa = x_prompt[b]
        else:
            xa = np.concatenate([np.zeros((256, D), np.float32), x_prompt[b][:SEQ - 256]], axis=0)
        tm = np.ones((128, NTA + NSMP), np.float32)
        if j == 0:
            tm[:, 0:2] = 0.0
        tm[SS:, NTA:] = 0.0
        xs = np.zeros((NSMP, 128, D), np.float32)
        sidx = [c * NSMP + s for s in range(NSMP)]
        for s, si in enumerate(sidx):
            xs[s, :SS] = x_sample[si]
        cvecs = np.stack([f(c_prompt)[b]] + [f(c_sample)[si] for si in sidx], axis=0)
        cT = np.ascontiguousarray(cvecs.reshape(1 + NSMP, 8, 128).transpose(2, 1, 0))
        m = dict(shared)
        m.update(xa=np.ascontiguousarray(xa), xs=xs.reshape(NSMP * 128, D), tmask=tm, ck=np.ascontiguousarray(ck[sidx]),
                 cv=np.ascontiguousarray(cv[sidx]), clf=np.ascontiguousarray(clf[sidx]), s0=np.ascontiguousarray(st[sidx]), cT=cT)
        in_maps.append(m)
    res = run_bass_kernel_spmd(nc, in_maps, core_ids=list(range(8)))
    yp = np.zeros((B, SEQ, D), np.float32)
    ys = np.zeros((SB, SS, D), np.float32)
    kp = np.zeros((1, B, SEQ, 8, 64), np.float32)
    vp = np.zeros((1, B, SEQ, 8, 64), np.float32)
    fp = np.zeros((1, B, SEQ, 8), np.float32)
    hp = np.zeros((1, B, 4, 128, 128), np.float32)
    ksm = np.zeros((1, SB, SS, 8, 64), np.float32)
    vsm = np.zeros((1, SB, SS, 8, 64), np.float32)
    fsm = np.zeros((1, SB, SS, 8), np.float32)
    hsm = np.zeros((1, SB, 4, 128, 128), np.float32)
    for c in range(8):
        r = res.results[c]
        b, j = c // 2, c % 2
        for o in range(NOWN):
            p = 2 * (o // 2) + 1
            g0 = p * 256 + (o % 2) * 128 - (256 if j == 0 else 0)
            sl = slice(o * 128, (o + 1) * 128)
            yp[b, g0:g0 + 128] = r["y"][sl]
            kp[0, b, g0:g0 + 128] = r["kn"][sl].reshape(128, 8, 64)
            vp[0, b, g0:g0 + 128] = r["vn"][sl].reshape(128, 8, 64)
            fp[0, b, g0:g0 + 128] = r["lf"][sl]
        if j == 1:
            hp[0, b] = r["hs"][0]
        for s in range(NSMP):
            si = c * NSMP + s
            r0 = (NOWN + s) * 128
            ys[si] = r["y"][r0:r0 + SS]
            ksm[0, si] = r["kn"][r0:r0 + SS].reshape(SS, 8, 64)
            vsm[0, si] = r["vn"][r0:r0 + SS].reshape(SS, 8, 64)
            fsm[0, si] = r["lf"][r0:r0 + SS]
            hsm[0, si] = r["hs"][1 + s]
    return (yp, ys, kp, vp, fp, hp, ksm, vsm, fsm, hsm)
```
